# Optimizing a Trainium2 kernel written in Bass

```python
import math
import jax, jax.numpy as jnp
from jax import lax
import numpy as np

D_MODEL = 1024
BATCH = 2
SEQ = 8192
DEPTH = 1

N_META = 16
GRID_W = 64
N_HEADS = 8
N_KV_HEADS = 2
HEAD_DIM = 64
Q_BLOCK = 128
ROPE_THETA = 10000.0
ATTN_WIDTH = N_HEADS * HEAD_DIM
KV_WIDTH = N_KV_HEADS * HEAD_DIM
SSM_WIDTH = D_MODEL // 2
SSM_GROUP = 16
SSM_GROUPS = SSM_WIDTH // SSM_GROUP
SSM_STATE = 64
DT_MIN = 1e-3
DT_MAX = 1e-1
N_EXPERTS = 16
EXPERT_FF = D_MODEL
CAPACITY_FACTOR = 2
IN_WIDTH = ATTN_WIDTH + 2 * KV_WIDTH + SSM_WIDTH + 2 * D_MODEL
SPLITS = (ATTN_WIDTH,
          ATTN_WIDTH + KV_WIDTH,
          ATTN_WIDTH + 2 * KV_WIDTH,
          ATTN_WIDTH + 2 * KV_WIDTH + SSM_WIDTH,
          ATTN_WIDTH + 2 * KV_WIDTH + SSM_WIDTH + D_MODEL)
DEEPNORM_ALPHA = (2.0 * DEPTH) ** 0.25
DEEPNORM_BETA = (8.0 * DEPTH) ** -0.25
LN_EPS = 1e-5
QK_EPS = 1e-6

kernel_name = "hybrid_gqa_s5_ecmoe_encoder"


def layer_norm(x, g, b):
    xf = x.astype(jnp.float32)
    mu = jnp.mean(xf, axis=-1, keepdims=True)
    var = jnp.mean(jnp.square(xf - mu), axis=-1, keepdims=True)
    y = (xf - mu) * lax.rsqrt(var + LN_EPS) * g.astype(jnp.float32) + b.astype(jnp.float32)
    return y.astype(x.dtype)


def rms_norm(x, g):
    xf = x.astype(jnp.float32)
    y = xf * lax.rsqrt(jnp.mean(jnp.square(xf), axis=-1, keepdims=True) + QK_EPS)
    return (y * g.astype(jnp.float32)).astype(x.dtype)


def axial_rope_angles(n_tokens):
    rows = n_tokens // GRID_W
    half = HEAD_DIM // 2
    inv_freq = ROPE_THETA ** (-jnp.arange(0, half, 2, dtype=jnp.float32) / half)
    row = jnp.repeat(jnp.arange(rows, dtype=jnp.float32), GRID_W)
    col = jnp.tile(jnp.arange(GRID_W, dtype=jnp.float32), rows)
    ang = jnp.concatenate([row[:, None] * inv_freq, col[:, None] * inv_freq], axis=-1)
    return jnp.concatenate([jnp.zeros((N_META, half), jnp.float32), ang], axis=0)


def apply_rope(x, cos, sin):
    xf = x.astype(jnp.float32).reshape(x.shape[:-1] + (HEAD_DIM // 2, 2))
    x0, x1 = xf[..., 0], xf[..., 1]
    c = cos[None, :, None, :]
    s = sin[None, :, None, :]
    out = jnp.stack([x0 * c - x1 * s, x0 * s + x1 * c], axis=-1)
    return out.reshape(x.shape).astype(x.dtype)


def block_attention(q, k, v):
    b_, l_ = q.shape[0], q.shape[1]
    n_blocks = -(-l_ // Q_BLOCK)
    lp = n_blocks * Q_BLOCK
    grp = N_HEADS // N_KV_HEADS
    qg = q.reshape(b_, l_, N_KV_HEADS, grp, HEAD_DIM)
    qg = jnp.pad(qg, ((0, 0), (0, lp - l_), (0, 0), (0, 0), (0, 0)))
    qb = qg.reshape(b_, n_blocks, Q_BLOCK, N_KV_HEADS, grp, HEAD_DIM).transpose(1, 0, 2, 3, 4, 5)
    scale = HEAD_DIM ** -0.5

    def one_block(qblk):
        s = jnp.einsum('bqkgd,bskd->bkgqs', qblk, k, preferred_element_type=jnp.float32) * scale
        p = jax.nn.softmax(s, axis=-1).astype(v.dtype)
        return jnp.einsum('bkgqs,bskd->bqkgd', p, v)

    ob = lax.map(one_block, qb)
    return ob.transpose(1, 0, 2, 3, 4, 5).reshape(b_, lp, ATTN_WIDTH)[:, :l_]


def zoh_discretise(a_re, a_im, log_dt, b_re, b_im):
    dt = jnp.exp(log_dt.astype(jnp.float32))[:, None]
    ar = a_re.astype(jnp.float32)
    ai = a_im.astype(jnp.float32)
    mag = jnp.exp(ar * dt)
    ang = ai * dt
    abar_re = mag * jnp.cos(ang)
    abar_im = mag * jnp.sin(ang)
    nr = abar_re - 1.0
    ni = abar_im
    den = ar * ar + ai * ai
    coef_re = (nr * ar + ni * ai) / den
    coef_im = (ni * ar - nr * ai) / den
    br = b_re.astype(jnp.float32)
    bi = b_im.astype(jnp.float32)
    bbar_re = coef_re[..., None] * br - coef_im[..., None] * bi
    bbar_im = coef_re[..., None] * bi + coef_im[..., None] * br
    return abar_re, abar_im, bbar_re, bbar_im


def complex_linear_scan(abar_re, abar_im, bu_re, bu_im):
    l_ = bu_re.shape[0]
    a_re = jnp.broadcast_to(abar_re, (l_,) + abar_re.shape)
    a_im = jnp.broadcast_to(abar_im, (l_,) + abar_im.shape)

    def combine(e1, e2):
        a1r, a1i, b1r, b1i = e1
        a2r, a2i, b2r, b2i = e2
        ar = a2r * a1r - a2i * a1i
        ai = a2r * a1i + a2i * a1r
        a2r_b = a2r[:, None]
        a2i_b = a2i[:, None]
        br = a2r_b * b1r - a2i_b * b1i + b2r
        bi = a2r_b * b1i + a2i_b * b1r + b2i
        return ar, ai, br, bi

    _, _, s_re, s_im = lax.associative_scan(combine, (a_re, a_im, bu_re, bu_im), axis=0)
    return s_re, s_im


def s5_branch(u, a_re, a_im, log_dt, b_re, b_im, c_re, c_im, d, w_glu, b_glu):
    b_, l_ = u.shape[0], u.shape[1]
    ug = u.reshape(b_, l_, SSM_GROUPS, SSM_GROUP).astype(jnp.float32)
    y = d.astype(jnp.float32) * ug
    for direction in range(2):
        abr, abi, bbr, bbi = zoh_discretise(a_re[direction], a_im[direction], log_dt[direction],
                                            b_re[direction], b_im[direction])
        src = ug if direction == 0 else jnp.flip(ug, axis=1)
        bu_re = jnp.einsum('blgh,gph->lbgp', src, bbr)
        bu_im = jnp.einsum('blgh,gph->lbgp', src, bbi)
        s_re, s_im = complex_linear_scan(abr, abi, bu_re, bu_im)
        yd = (jnp.einsum('lbgp,ghp->blgh', s_re, c_re[direction].astype(jnp.float32))
              - jnp.einsum('lbgp,ghp->blgh', s_im, c_im[direction].astype(jnp.float32)))
        if direction == 1:
            yd = jnp.flip(yd, axis=1)
        y = y + yd
    y = jax.nn.gelu(y.reshape(b_, l_, SSM_WIDTH))
    y = y * jax.nn.sigmoid(y @ w_glu.astype(jnp.float32) + b_glu.astype(jnp.float32))
    return y.astype(u.dtype)


def expert_choice_moe(t, w_router, w_gate, w_up, w_down):
    b_, s_, d_ = t.shape
    cap = CAPACITY_FACTOR * s_ // N_EXPERTS
    logits = jnp.einsum('bsd,de->bes', t, w_router, preferred_element_type=jnp.float32)
    affinity = jax.nn.softmax(logits, axis=1)
    gate, idx = lax.top_k(affinity, cap)
    xe = jax.vmap(lambda tb, ib: tb[ib])(t, idx)
    h = (jax.nn.silu(jnp.einsum('becd,edf->becf', xe, w_gate))
         * jnp.einsum('becd,edf->becf', xe, w_up))
    ye = jnp.einsum('becf,efd->becd', h, w_down) * gate[..., None].astype(t.dtype)
    out = jax.vmap(lambda ib, yb: jnp.zeros((s_, d_), t.dtype).at[ib.reshape(-1)].add(
        yb.reshape(-1, d_)))(idx, ye)
    return out


def setup_inputs(seed: int = 0) -> dict:
    key = jax.random.key(seed)
    ks = jax.random.split(key, 32)
    f32 = jnp.float32
    n = lambda k, shape: jax.random.normal(k, shape, f32)
    P = SSM_STATE
    G = SSM_GROUPS
    H = SSM_GROUP
    a_im_init = math.pi * jnp.arange(P, dtype=f32)
    return {
        "x": n(ks[0], (BATCH, SEQ, D_MODEL)),
        "meta_tokens": n(ks[1], (N_META, D_MODEL)),
        "ln_in_g": 1.0 + 0.02 * n(ks[2], (D_MODEL,)),
        "ln_in_b": 0.02 * n(ks[3], (D_MODEL,)),
        "w_in": n(ks[4], (DEPTH, D_MODEL, IN_WIDTH)) * D_MODEL ** -0.5,
        "q_norm_g": 1.0 + 0.02 * n(ks[5], (DEPTH, HEAD_DIM)),
        "k_norm_g": 1.0 + 0.02 * n(ks[6], (DEPTH, HEAD_DIM)),
        "ssm_a_re": -0.5 + 0.01 * n(ks[7], (DEPTH, 2, G, P)),
        "ssm_a_im": a_im_init + 0.01 * n(ks[8], (DEPTH, 2, G, P)),
        "ssm_log_dt": jax.random.uniform(ks[9], (DEPTH, 2, G), f32,
                                         math.log(DT_MIN), math.log(DT_MAX)),
        "ssm_b_re": n(ks[10], (DEPTH, 2, G, P, H)) * (2 * H) ** -0.5,
        "ssm_b_im": n(ks[11], (DEPTH, 2, G, P, H)) * (2 * H) ** -0.5,
        "ssm_c_re": n(ks[12], (DEPTH, 2, G, H, P)) * P ** -0.5,
        "ssm_c_im": n(ks[13], (DEPTH, 2, G, H, P)) * P ** -0.5,
        "ssm_d": n(ks[14], (DEPTH, G, H)),
        "w_glu": n(ks[15], (DEPTH, SSM_WIDTH, SSM_WIDTH)) * SSM_WIDTH ** -0.5,
        "b_glu": 0.02 * n(ks[16], (DEPTH, SSM_WIDTH)),
        "w_attn_br": n(ks[17], (DEPTH, ATTN_WIDTH, D_MODEL)) * ATTN_WIDTH ** -0.5,
        "w_ssm_br": n(ks[18], (DEPTH, SSM_WIDTH, D_MODEL)) * SSM_WIDTH ** -0.5,
        "w_o": n(ks[19], (DEPTH, D_MODEL, D_MODEL)) * D_MODEL ** -0.5 * DEEPNORM_BETA,
        "ln1_g": 1.0 + 0.02 * n(ks[20], (DEPTH, D_MODEL)),
        "ln1_b": 0.02 * n(ks[21], (DEPTH, D_MODEL)),
        "w_router": n(ks[22], (DEPTH, D_MODEL, N_EXPERTS)) * D_MODEL ** -0.5,
        "w_gate_e": n(ks[23], (DEPTH, N_EXPERTS, D_MODEL, EXPERT_FF)) * D_MODEL ** -0.5,
        "w_up_e": n(ks[24], (DEPTH, N_EXPERTS, D_MODEL, EXPERT_FF)) * D_MODEL ** -0.5,
        "w_down_e": n(ks[25], (DEPTH, N_EXPERTS, EXPERT_FF, D_MODEL)) * EXPERT_FF ** -0.5 * DEEPNORM_BETA,
        "ln2_g": 1.0 + 0.02 * n(ks[26], (DEPTH, D_MODEL)),
        "ln2_b": 0.02 * n(ks[27], (DEPTH, D_MODEL)),
    }


def reference(x, meta_tokens, ln_in_g, ln_in_b, w_in, q_norm_g, k_norm_g,
              ssm_a_re, ssm_a_im, ssm_log_dt, ssm_b_re, ssm_b_im, ssm_c_re, ssm_c_im,
              ssm_d, w_glu, b_glu, w_attn_br, w_ssm_br, w_o, ln1_g, ln1_b,
              w_router, w_gate_e, w_up_e, w_down_e, ln2_g, ln2_b):
    b_, s_, d_ = x.shape
    l_ = s_ + N_META
    meta = jnp.broadcast_to(meta_tokens.astype(x.dtype)[None], (b_, N_META, d_))
    h = layer_norm(jnp.concatenate([meta, x], axis=1), ln_in_g, ln_in_b)
    ang = axial_rope_angles(s_)
    cos, sin = jnp.cos(ang), jnp.sin(ang)
    for l in range(DEPTH):
        proj = h @ w_in[l]
        q, k, v, u, g_attn, g_ssm = jnp.split(proj, SPLITS, axis=-1)
        q = apply_rope(rms_norm(q.reshape(b_, l_, N_HEADS, HEAD_DIM), q_norm_g[l]), cos, sin)
        k = apply_rope(rms_norm(k.reshape(b_, l_, N_KV_HEADS, HEAD_DIM), k_norm_g[l]), cos, sin)
        v = v.reshape(b_, l_, N_KV_HEADS, HEAD_DIM)
        y_attn = block_attention(q, k, v)
        y_ssm = s5_branch(u, ssm_a_re[l], ssm_a_im[l], ssm_log_dt[l], ssm_b_re[l], ssm_b_im[l],
                          ssm_c_re[l], ssm_c_im[l], ssm_d[l], w_glu[l], b_glu[l])
        merged = (jax.nn.sigmoid(g_attn) * (y_attn @ w_attn_br[l])
                  + jax.nn.sigmoid(g_ssm) * (y_ssm @ w_ssm_br[l]))
        h = layer_norm(DEEPNORM_ALPHA * h + merged @ w_o[l], ln1_g[l], ln1_b[l])
        moe = expert_choice_moe(h[:, N_META:], w_router[l], w_gate_e[l], w_up_e[l], w_down_e[l])
        moe = jnp.concatenate([jnp.zeros((b_, N_META, d_), h.dtype), moe], axis=1)
        h = layer_norm(DEEPNORM_ALPHA * h + moe, ln2_g[l], ln2_b[l])
    return h[:, N_META:]
```

```python
import contextlib
import numpy as np
import ml_dtypes
import concourse.bass as bass
import concourse.mybir as mybir
from concourse.bass_utils import run_bass_kernel_spmd

F32 = mybir.dt.float32
BF16 = mybir.dt.bfloat16
ALU = mybir.AluOpType
AF = mybir.ActivationFunctionType
AX = mybir.AxisListType
NPBF = ml_dtypes.bfloat16

NT = 17
TOK = 2048
NCH = 129
ALPHA = 2.0 ** 0.25


class Buf:
    __slots__ = ("name", "w", "rs")

    def __init__(self, name="b"):
        self.name = name
        self.w = None
        self.rs = []


class Sched:
    ENG = ("pe", "act", "dve", "pool", "sp")

    def __init__(self, nc, stack):
        self.nc = nc
        self.prog = {e: [] for e in self.ENG}
        self.sems = {}
        self.cnt = {}
        self.seen = {e: {} for e in self.ENG}
        self.stack = stack
        for e in self.ENG:
            self._sem("E_" + e)

    def _sem(self, key):
        if key not in self.sems:
            self.sems[key] = self.stack.enter_context(self.nc.semaphore(key))
            self.cnt[key] = 0
        return self.sems[key]

    def _need(self, eng, need):
        for k, c in need.items():
            if k.startswith("D_"):
                c = self.cnt[k]
            if self.seen[eng].get(k, 0) < c:
                self.seen[eng][k] = c
                self.prog[eng].append(("wait", k, c))

    def _deps(self, eng, reads, writes):
        need = {}

        def add(d):
            if d is None:
                return
            k, c = d
            if k == "E_pe" and eng == "pe":
                return
            if need.get(k, 0) < c:
                need[k] = c
        for b in reads:
            add(b.w)
        for b in writes:
            add(b.w)
            for r in b.rs:
                add(r)
        self._need(eng, need)

    def _mark(self, tag, reads, writes):
        for b in reads:
            b.rs.append(tag)
            if len(b.rs) > 64:
                best = {}
                for k, c in b.rs:
                    best[k] = max(best.get(k, 0), c)
                b.rs = list(best.items())
        for b in writes:
            b.w = tag
            b.rs = []

    def op(self, eng, fn, reads=(), writes=()):
        self._deps(eng, reads, writes)
        key = "E_" + eng
        self.cnt[key] += 1
        self.prog[eng].append(("op", fn, key, 1))
        self._mark((key, self.cnt[key]), reads, writes)

    def dma(self, eng, stream, fn, reads=(), writes=()):
        self._deps(eng, reads, writes)
        key = "D_" + stream
        self._sem(key)
        self.cnt[key] += 16
        self.prog[eng].append(("op", fn, key, 16))
        self._mark((key, self.cnt[key]), reads, writes)

    def barrier(self):
        need = {k: c for k, c in self.cnt.items() if c > 0}
        for e in self.ENG:
            self._need(e, dict(need))

    def emit(self, block):
        S = self

        def runner(e):
            def run(h):
                for it in S.prog[e]:
                    if it[0] == "wait":
                        h.wait_ge(S.sems[it[1]], it[2])
                    else:
                        _, fn, key, inc = it
                        fn(h).then_inc(S.sems[key], inc)
            return run
        block.tensor(runner("pe"))
        block.scalar(runner("act"))
        block.vector(runner("dve"))
        block.gpsimd(runner("pool"))
        block.sync(runner("sp"))


class K:
    def __init__(self):
        self.nc = bass.Bass("TRN2", target_bir_lowering=False)
        self.top = contextlib.ExitStack()
        self.S = Sched(self.nc, self.top)
        self.din = {}
        self.dout = {}
        self.n = 0

    def inp(self, name, shape, dt=F32):
        self.din[name] = self.nc.dram_tensor(name, list(shape), dt, kind="ExternalInput").ap()
        return self.din[name]

    def out(self, name, shape, dt=F32):
        self.dout[name] = self.nc.dram_tensor(name, list(shape), dt, kind="ExternalOutput").ap()
        return self.dout[name]

    def sb(self, st, shape, dt=F32, name=None):
        self.n += 1
        return st.enter_context(self.nc.sbuf_tensor(f"{name or 's'}{self.n}", list(shape), dt))

    def ps(self, st, dt=F32, name=None):
        self.n += 1
        n = 512 if dt == F32 else 1024
        return st.enter_context(self.nc.psum_tensor(f"{name or 'p'}{self.n}", [128, n], dt))


def bc(ap, shape):
    return ap.to_broadcast(list(shape))


def stage1(k, P, want_h0):
    nc, S = k.nc, k.S
    D = k.din
    with contextlib.ExitStack() as st:
        ident_f = P["ident_f"]
        g_t = k.sb(st, [128, 1024])
        b_t = k.sb(st, [128, 1024])
        qkg = k.sb(st, [128, 640])
        wst = [k.sb(st, [128, 1280]) for _ in range(2)]
        xt = [k.sb(st, [128, 1024]) for _ in range(2)]
        hf = [k.sb(st, [128, 1024]) for _ in range(2)]
        hb = [k.sb(st, [128, 1024], BF16) for _ in range(2)]
        st6 = k.sb(st, [128, 2, 6])
        mv = k.sb(st, [128, 2])
        rstd = k.sb(st, [128, 1])
        qk = k.sb(st, [128, 640])
        sq = k.sb(st, [128, 640])
        ss = k.sb(st, [128, 10])
        cs = [k.sb(st, [128, 2, 32]) for _ in range(2)]
        t1 = k.sb(st, [128, 10, 32])
        t2 = k.sb(st, [128, 10, 32])
        qr = k.sb(st, [128, 640], BF16)
        pT = k.ps(st, BF16)
        pA = k.ps(st)
        pB = k.ps(st)
        pU = k.ps(st)
        pQ = k.ps(st, BF16)
        B = {n: Buf(n) for n in "id g b qkg w wst0 wst1 x0 x1 hf0 hf1 hb0 hb1 stat qk sq ss cs0 cs1 t qr pT pA pB pU pQ h0d".split()}
        ident_b, qT, kT, vv, uT = P["ident_b"], P["qT"], P["kTo"], P["vo"], P["uT"]
        BP = P["B"]
        w_bf = k.sb(st, [128, 8, 1280], BF16)
        hTs = [k.sb(st, [128, 8, 128], BF16) for _ in range(2)]
        bhT = [Buf(), Buf()]
        BP["w_bf"] = Buf()

        S.dma("sp", "c", lambda e: e.dma_start(out=ident_f[:], in_=D["ident"]), writes=[B["id"], P["B"]["ident_f"]])
        S.dma("sp", "c", lambda e: e.dma_start(out=g_t[:], in_=D["ln_in_g"].partition_broadcast(128)), writes=[B["g"]])
        S.dma("sp", "c", lambda e: e.dma_start(out=b_t[:], in_=D["ln_in_b"].partition_broadcast(128)), writes=[B["b"]])
        S.dma("sp", "c", lambda e: e.dma_start(out=qkg[:], in_=D["qkg"].partition_broadcast(128)), writes=[B["qkg"]])
        S.op("act", lambda e: e.copy(out=ident_b[:], in_=ident_f[:]), reads=[B["id"]], writes=[BP["ident_b"]])
        for kk in range(8):
            s = kk % 2
            S.dma("sp", f"w{s}", lambda e, kk=kk, s=s: e.dma_start(out=wst[s][:], in_=D["w_qkvu"][kk * 128:(kk + 1) * 128, :]),
                  writes=[B[f"wst{s}"]])
            S.op("act", lambda e, kk=kk, s=s: e.copy(out=w_bf[:, kk, :], in_=wst[s][:]), reads=[B[f"wst{s}"]], writes=[BP["w_bf"]])

        for t in range(NT):
            s = t % 2
            bx, bhf, bhb, bcs = B[f"x{s}"], B[f"hf{s}"], B[f"hb{s}"], B[f"cs{s}"]
            S.dma("sp", f"x{s}", lambda e, t=t, s=s: e.dma_start(out=xt[s][:], in_=D["xin"][t * 128:(t + 1) * 128, :]), writes=[bx])
            S.dma("sp", f"cs{s}", lambda e, t=t, s=s: e.dma_start(out=cs[s][:], in_=D["rope"][t * 128:(t + 1) * 128, :, :]), writes=[bcs])
            for c in range(2):
                S.op("dve", lambda e, c=c, s=s: e.bn_stats(out=st6[:, c, :], in_=xt[s][:, c * 512:(c + 1) * 512]),
                     reads=[bx], writes=[B["stat"]])
            S.op("dve", lambda e: e.bn_aggr(out=mv[:], in_=st6[:]), reads=[B["stat"]], writes=[B["stat"]])
            S.op("act", lambda e: e.activation(out=rstd[:], in_=mv[:, 1:2], func=AF.Sqrt, bias=1e-5, scale=1.0),
                 reads=[B["stat"]], writes=[B["stat"]])
            S.op("dve", lambda e: e.reciprocal(out=rstd[:], in_=rstd[:]), reads=[B["stat"]], writes=[B["stat"]])
            S.op("dve", lambda e, s=s: e.tensor_scalar(out=hf[s][:], in0=xt[s][:], scalar1=mv[:, 0:1], scalar2=rstd[:, 0:1],
                                                       op0=ALU.subtract, op1=ALU.mult), reads=[bx, B["stat"]], writes=[bhf])
            S.op("pool", lambda e, s=s: e.tensor_tensor(out=hf[s][:], in0=hf[s][:], in1=g_t[:], op=ALU.mult),
                 reads=[bhf, B["g"]], writes=[bhf])
            S.op("pool", lambda e, s=s: e.tensor_tensor(out=hf[s][:], in0=hf[s][:], in1=b_t[:], op=ALU.add),
                 reads=[bhf, B["b"]], writes=[bhf])
            if want_h0 and t >= 1:
                S.dma("sp", "h0", lambda e, t=t, s=s: e.dma_start(out=P["h0_d"][(t - 1) * 128:t * 128, :], in_=hf[s][:]),
                      reads=[bhf], writes=[B["h0d"], BP["h0_d"]])
            S.op("act", lambda e, s=s: e.copy(out=hb[s][:], in_=hf[s][:]), reads=[bhf], writes=[bhb])
            for kk in range(8):
                S.op("pe", lambda e, kk=kk, s=s: e.transpose(out=pT[:, kk * 128:(kk + 1) * 128], in_=hb[s][:, kk * 128:(kk + 1) * 128],
                                                            identity=ident_b[:]), reads=[bhb, BP["ident_b"]], writes=[B["pT"]])
            hT = hTs[s]
            S.op("dve", lambda e, hT=hT: e.tensor_copy(out=hT[:], in_=pT[:].rearrange("p (k n) -> p k n", k=8)),
                 reads=[B["pT"]], writes=[bhT[s]])
            S.dma("sp", "hTd", lambda e, t=t, hT=hT: e.dma_start(out=P["hT_d"][:, :, t * 128:(t + 1) * 128], in_=hT[:]),
                  reads=[bhT[s]], writes=[BP["hT_d"]])
            for kk in range(8):
                S.op("pe", lambda e, kk=kk, t=t, hT=hT: e.matmul(pA[:], lhsT=hT[:, kk, :], rhs=w_bf[:, kk, 0:512],
                                                         start=(kk == 0), stop=(kk == 7)), reads=[bhT[s], BP["w_bf"]], writes=[B["pA"]])
            for kk in range(8):
                S.op("pe", lambda e, kk=kk, t=t, hT=hT: e.matmul(pB[:, 0:256], lhsT=hT[:, kk, :], rhs=w_bf[:, kk, 512:768],
                                                         start=(kk == 0), stop=(kk == 7)), reads=[bhT[s], BP["w_bf"]], writes=[B["pB"]])
            for blk in range(4):
                for kk in range(8):
                    S.op("pe", lambda e, kk=kk, t=t, blk=blk, hT=hT: e.matmul(
                        pU[:, blk * 128:(blk + 1) * 128], lhsT=w_bf[:, kk, 768 + blk * 128:768 + (blk + 1) * 128],
                        rhs=hT[:, kk, :], start=(kk == 0), stop=(kk == 7)),
                        reads=[bhT[s], BP["w_bf"]], writes=[B["pU"]])
            if t == 0:
                S.op("act", lambda e: e.copy(out=uT[:, :, :, 0:1],
                                             in_=pU[:].rearrange("p (b c i) -> p b i c", b=4, i=16)[:, :, :, 0:1]),
                     reads=[B["pU"]], writes=[BP["uT"]])
            else:
                c0 = 1 + 8 * (t - 1)
                S.op("act", lambda e, c0=c0: e.copy(out=uT[:, :, :, c0:c0 + 8],
                                                    in_=pU[:].rearrange("p (b c i) -> p b i c", b=4, i=16)),
                     reads=[B["pU"]], writes=[BP["uT"]])
            S.op("act", lambda e: e.copy(out=qk[:, 0:512], in_=pA[:]), reads=[B["pA"]], writes=[B["qk"]])
            S.op("act", lambda e: e.copy(out=qk[:, 512:640], in_=pB[:, 0:128]), reads=[B["pB"]], writes=[B["qk"]])
            S.op("act", lambda e, t=t: e.copy(out=vv[:, t, :, 0:64], in_=pB[:, 128:256].rearrange("p (h d) -> p h d", h=2)),
                 reads=[B["pB"]], writes=[BP["vo"]])
            S.op("dve", lambda e: e.tensor_tensor(out=sq[:], in0=qk[:], in1=qk[:], op=ALU.mult), reads=[B["qk"]], writes=[B["sq"]])
            S.op("dve", lambda e: e.tensor_reduce(out=ss[:], in_=sq[:].rearrange("p (h d) -> p h d", h=10), axis=AX.X, op=ALU.add),
                 reads=[B["sq"]], writes=[B["ss"]])
            S.op("act", lambda e: e.activation(out=ss[:], in_=ss[:], func=AF.Sqrt, bias=1e-6, scale=1.0 / 64), reads=[B["ss"]], writes=[B["ss"]])
            S.op("dve", lambda e: e.reciprocal(out=ss[:], in_=ss[:]), reads=[B["ss"]], writes=[B["ss"]])
            S.op("dve", lambda e: e.tensor_tensor(out=sq[:].rearrange("p (h d) -> p h d", h=10), in0=qk[:].rearrange("p (h d) -> p h d", h=10),
                                                  in1=bc(ss[:].unsqueeze(2), [128, 10, 64]), op=ALU.mult),
                 reads=[B["qk"], B["ss"]], writes=[B["sq"]])
            S.op("dve", lambda e: e.tensor_tensor(out=sq[:], in0=sq[:], in1=qkg[:], op=ALU.mult), reads=[B["sq"], B["qkg"]], writes=[B["sq"]])
            x4 = sq[:].rearrange("p (h i two) -> p h i two", h=10, two=2)
            o4 = qr[:].rearrange("p (h i two) -> p h i two", h=10, two=2)
            cb = lambda s=s: bc(cs[s][:, 0:1, :], [128, 10, 32])
            sb_ = lambda s=s: bc(cs[s][:, 1:2, :], [128, 10, 32])
            S.op("dve", lambda e, s=s: e.tensor_tensor(out=t1[:], in0=x4[:, :, :, 0], in1=bc(cs[s][:, 0:1, :], [128, 10, 32]), op=ALU.mult),
                 reads=[B["sq"], bcs], writes=[B["t"]])
            S.op("dve", lambda e, s=s: e.tensor_tensor(out=t2[:], in0=x4[:, :, :, 1], in1=bc(cs[s][:, 1:2, :], [128, 10, 32]), op=ALU.mult),
                 reads=[B["sq"], bcs], writes=[B["t"]])
            S.op("dve", lambda e: e.tensor_tensor(out=o4[:, :, :, 0], in0=t1[:], in1=t2[:], op=ALU.subtract), reads=[B["t"]], writes=[B["qr"]])
            S.op("dve", lambda e, s=s: e.tensor_tensor(out=t1[:], in0=x4[:, :, :, 0], in1=bc(cs[s][:, 1:2, :], [128, 10, 32]), op=ALU.mult),
                 reads=[B["sq"], bcs, B["qr"]], writes=[B["t"]])
            S.op("dve", lambda e, s=s: e.tensor_tensor(out=t2[:], in0=x4[:, :, :, 1], in1=bc(cs[s][:, 0:1, :], [128, 10, 32]), op=ALU.mult),
                 reads=[B["sq"], bcs], writes=[B["t"]])
            S.op("dve", lambda e: e.tensor_tensor(out=o4[:, :, :, 1], in0=t1[:], in1=t2[:], op=ALU.add), reads=[B["t"]], writes=[B["qr"]])
            for j in range(5):
                S.op("pe", lambda e, j=j: e.transpose(out=pQ[:, j * 128:(j + 1) * 128], in_=qr[:, j * 128:(j + 1) * 128], identity=ident_b[:]),
                     reads=[B["qr"], BP["ident_b"]], writes=[B["pQ"]])
            S.op("act", lambda e, t=t: e.copy(out=qT[:, :, t * 128:(t + 1) * 128], in_=pQ[:, 0:512].rearrange("p (j n) -> p j n", j=4)),
                 reads=[B["pQ"]], writes=[BP["qT"]])
            S.op("act", lambda e, t=t: e.copy(out=kT[:, t * 128:(t + 1) * 128], in_=pQ[:, 512:640]), reads=[B["pQ"]], writes=[BP["kTo"]])
        S.barrier()


def attention(k, P):
    nc, S = k.nc, k.S
    D = k.din
    BP = P["B"]
    qT, ident_b = P["qT"], P["ident_b"]
    with contextlib.ExitStack() as st:
        kTa = k.sb(st, [128, 65 * 128], BF16)
        va = k.sb(st, [128, 65, 2, 65], BF16)
        pt = [k.sb(st, [128, 512], BF16) for _ in range(2)]
        ya = [k.sb(st, [128, 512], BF16) for _ in range(4)]
        rinv = k.sb(st, [128, 4])
        yo = [k.sb(st, [128, 4, 128], BF16) for _ in range(2)]
        byo = [Buf(), Buf()]
        pS = [k.ps(st) for _ in range(2)]
        pO = [k.ps(st) for _ in range(4)]
        pT = k.ps(st, BF16)
        bk, bv, brin, bpT = Buf(), Buf(), Buf(), Buf()
        bpS = [Buf(), Buf()]
        bpt = [Buf(), Buf()]
        bpO = [Buf() for _ in range(4)]
        bya = [Buf() for _ in range(4)]
        S.dma("sp", "kv", lambda e: e.dma_start(out=kTa[:], in_=D["kT_all"]), writes=[bk])
        S.dma("sp", "kv", lambda e: e.dma_start(out=va[:], in_=D["v_all"]), writes=[bv])
        it = 0
        for qb in range(4):
            q0 = 128 + qb * 512
            for head in range(8):
                half, hi = head // 4, head % 4
                r0 = half * 64
                for kt in range(65):
                    nk = 128 if kt < 64 else 16
                    sl = it % 2
                    it += 1
                    S.op("pe", lambda e, sl=sl, kt=kt, nk=nk, r0=r0, hi=hi, q0=q0: e.matmul(
                        pS[sl][0:nk, :], lhsT=kTa[r0:r0 + 64, kt * 128:kt * 128 + nk], rhs=qT[r0:r0 + 64, hi, q0:q0 + 512],
                        start=True, stop=True), reads=[bk, BP["qT"]], writes=[bpS[sl]])
                    S.op("act", lambda e, sl=sl, nk=nk: e.activation(out=pt[sl][0:nk, :], in_=pS[sl][0:nk, :], func=AF.Exp, scale=0.125),
                         reads=[bpS[sl]], writes=[bpt[sl]])
                    for sub in range(4):
                        S.op("pe", lambda e, sl=sl, kt=kt, nk=nk, sub=sub, half=half: e.matmul(
                            pO[sub][:, 0:65], lhsT=pt[sl][0:nk, sub * 128:(sub + 1) * 128], rhs=va[0:nk, kt, half, :],
                            start=(kt == 0), stop=(kt == 64)), reads=[bpt[sl], bv], writes=[bpO[sub]])
                for sub in range(4):
                    S.op("dve", lambda e, sub=sub: e.reciprocal(out=rinv[:, sub:sub + 1], in_=pO[sub][:, 64:65]),
                         reads=[bpO[sub]], writes=[brin])
                    S.op("dve", lambda e, sub=sub, head=head: e.tensor_scalar(
                        out=ya[sub][:, head * 64:(head + 1) * 64], in0=pO[sub][:, 0:64], scalar1=rinv[:, sub:sub + 1], scalar2=None,
                        op0=ALU.mult), reads=[bpO[sub], brin], writes=[bya[sub]])
            for sub in range(4):
                for j in range(4):
                    S.op("pe", lambda e, sub=sub, j=j: e.transpose(out=pT[:, j * 128:(j + 1) * 128], in_=ya[sub][:, j * 128:(j + 1) * 128],
                                                                  identity=ident_b[:]), reads=[bya[sub], BP["ident_b"]], writes=[bpT])
                c0 = qb * 512 + sub * 128
                so = sub % 2
                S.op("act", lambda e, so=so: e.copy(out=yo[so][:], in_=pT[:, 0:512].rearrange("p (j n) -> p j n", j=4)),
                     reads=[bpT], writes=[byo[so]])
                S.dma("sp", f"yo{so}", lambda e, c0=c0, so=so: e.dma_start(out=P["yaT_d"][:, :, c0:c0 + 128], in_=yo[so][:]),
                      reads=[byo[so]], writes=[BP["yaT_d"]])
        S.barrier()


NE = 51
TWO_PI = 6.283185307179586


def ssm(k, P, final):
    nc, S = k.nc, k.S
    D = k.din
    BP = P["B"]
    uT, ident_b, ident_f = P["uT"], P["ident_b"], P["ident_f"]
    V = lambda fn, r, w: S.op("dve", fn, reads=r, writes=w)
    A = lambda fn, r, w: S.op("act", fn, reads=r, writes=w)
    G = lambda fn, r, w: S.op("pool", fn, reads=r, writes=w)
    T = lambda fn, r, w: S.op("pe", fn, reads=r, writes=w)
    with contextlib.ExitStack() as st:
        are = k.sb(st, [128, 32]); aim = k.sb(st, [128, 32]); dt = k.sb(st, [128, 32])
        Bre = k.sb(st, [128, 32, 16]); Bim = k.sb(st, [128, 32, 16])
        Cre = k.sb(st, [128, 32, 16]); Cim = k.sb(st, [128, 32, 16])
        Eall = k.sb(st, [128, NE]); mask8 = k.sb(st, [128, 8]); dvec = k.sb(st, [128, 4])
        mk = k.sb(st, [128, 4])
        bl = Buf("ld")
        for tl, nm in [(are, "s_are"), (aim, "s_aim"), (dt, "s_ldt"), (Bre, "s_bre"), (Bim, "s_bim"), (Cre, "s_cre"), (Cim, "s_cim"),
                       (Eall, "s_eall"), (mask8, "s_mask8"), (dvec, "s_dvec"), (mk, "s_mk")]:
            S.dma("sp", "c", lambda e, tl=tl, nm=nm: e.dma_start(out=tl[:], in_=D[nm]), writes=[bl])
        ardt = k.sb(st, [128, 32]); aidt = k.sb(st, [128, 32])
        Pre = k.sb(st, [128, 32, NE]); Pim = k.sb(st, [128, 32, NE])
        bp = Buf("pow")
        with contextlib.ExitStack() as stp:
            tq = k.sb(stp, [128, 32, NE]); tn = k.sb(stp, [128, 32, NE]); ti = k.sb(stp, [128, 32, NE], mybir.dt.int32)
            mag = k.sb(stp, [128, 32, NE])
            A(lambda e: e.activation(out=dt[:], in_=dt[:], func=AF.Exp), [bl], [bp])
            V(lambda e: e.tensor_tensor(out=ardt[:], in0=are[:], in1=dt[:], op=ALU.mult), [bl, bp], [bp])
            V(lambda e: e.tensor_tensor(out=aidt[:], in0=aim[:], in1=dt[:], op=ALU.mult), [bl, bp], [bp])
            sh = [128, 32, NE]
            eb = lambda: bc(Eall[:].unsqueeze(1), sh)
            V(lambda e: e.tensor_tensor(out=mag[:], in0=bc(ardt[:].unsqueeze(2), sh), in1=eb(), op=ALU.mult), [bp, bl], [bp])
            A(lambda e: e.activation(out=mag[:], in_=mag[:], func=AF.Exp), [bp], [bp])
            V(lambda e: e.tensor_tensor(out=tq[:], in0=bc(aidt[:].unsqueeze(2), sh), in1=eb(), op=ALU.mult), [bp, bl], [bp])
            V(lambda e: e.tensor_scalar(out=tq[:], in0=tq[:], scalar1=1.0 / TWO_PI, scalar2=None, op0=ALU.mult), [bp], [bp])
            V(lambda e: e.tensor_copy(out=ti[:], in_=tq[:]), [bp], [bp])
            V(lambda e: e.tensor_copy(out=tn[:], in_=ti[:]), [bp], [bp])
            V(lambda e: e.tensor_tensor(out=tq[:], in0=tq[:], in1=tn[:], op=ALU.subtract), [bp], [bp])
            V(lambda e: e.tensor_scalar(out=tn[:], in0=tq[:], scalar1=0.5, scalar2=None, op0=ALU.is_gt), [bp], [bp])
            V(lambda e: e.tensor_tensor(out=tq[:], in0=tq[:], in1=tn[:], op=ALU.subtract), [bp], [bp])
            A(lambda e: e.activation(out=Pim[:], in_=tq[:], func=AF.Sin, scale=TWO_PI), [bp], [bp])
            V(lambda e: e.tensor_scalar(out=tq[:], in0=tq[:], scalar1=0.25, scalar2=None, op0=ALU.add), [bp], [bp])
            V(lambda e: e.tensor_scalar(out=tn[:], in0=tq[:], scalar1=0.5, scalar2=None, op0=ALU.is_gt), [bp], [bp])
            V(lambda e: e.tensor_tensor(out=tq[:], in0=tq[:], in1=tn[:], op=ALU.subtract), [bp], [bp])
            A(lambda e: e.activation(out=Pre[:], in_=tq[:], func=AF.Sin, scale=TWO_PI), [bp], [bp])
            V(lambda e: e.tensor_tensor(out=Pre[:], in0=Pre[:], in1=mag[:], op=ALU.mult), [bp], [bp])
            V(lambda e: e.tensor_tensor(out=Pim[:], in0=Pim[:], in1=mag[:], op=ALU.mult), [bp], [bp])
            S.barrier()
        cre = k.sb(st, [128, 32]); cim = k.sb(st, [128, 32]); den = k.sb(st, [128, 32]); tA = k.sb(st, [128, 32]); tB = k.sb(st, [128, 32])
        nr = k.sb(st, [128, 32])
        V(lambda e: e.tensor_scalar(out=nr[:], in0=Pre[:, :, 50], scalar1=-1.0, scalar2=None, op0=ALU.add), [bp], [bp])
        V(lambda e: e.tensor_tensor(out=den[:], in0=are[:], in1=are[:], op=ALU.mult), [bp, bl], [bp])
        V(lambda e: e.tensor_tensor(out=tA[:], in0=aim[:], in1=aim[:], op=ALU.mult), [bp, bl], [bp])
        V(lambda e: e.tensor_tensor(out=den[:], in0=den[:], in1=tA[:], op=ALU.add), [bp], [bp])
        V(lambda e: e.reciprocal(out=den[:], in_=den[:]), [bp], [bp])
        V(lambda e: e.tensor_tensor(out=tA[:], in0=nr[:], in1=are[:], op=ALU.mult), [bp], [bp])
        V(lambda e: e.tensor_tensor(out=tB[:], in0=Pim[:, :, 50], in1=aim[:], op=ALU.mult), [bp], [bp])
        V(lambda e: e.tensor_tensor(out=tA[:], in0=tA[:], in1=tB[:], op=ALU.add), [bp], [bp])
        V(lambda e: e.tensor_tensor(out=cre[:], in0=tA[:], in1=den[:], op=ALU.mult), [bp], [bp])
        V(lambda e: e.tensor_tensor(out=tA[:], in0=Pim[:, :, 50], in1=are[:], op=ALU.mult), [bp], [bp])
        V(lambda e: e.tensor_tensor(out=tB[:], in0=nr[:], in1=aim[:], op=ALU.mult), [bp], [bp])
        V(lambda e: e.tensor_tensor(out=tA[:], in0=tA[:], in1=tB[:], op=ALU.subtract), [bp], [bp])
        V(lambda e: e.tensor_tensor(out=cim[:], in0=tA[:], in1=den[:], op=ALU.mult), [bp], [bp])
        Gre = k.sb(st, [128, 32, 32]); Gim = k.sb(st, [128, 32, 32])
        s32 = [128, 32, 32]
        with contextlib.ExitStack() as stp:
            g1 = k.sb(stp, [128, 32, 32])
            V(lambda e: e.tensor_tensor(out=Gre[:], in0=Pre[:, :, 0:32], in1=bc(cre[:].unsqueeze(2), s32), op=ALU.mult), [bp], [bp])
            V(lambda e: e.tensor_tensor(out=g1[:], in0=Pim[:, :, 0:32], in1=bc(cim[:].unsqueeze(2), s32), op=ALU.mult), [bp], [bp])
            V(lambda e: e.tensor_tensor(out=Gre[:], in0=Gre[:], in1=g1[:], op=ALU.subtract), [bp], [bp])
            V(lambda e: e.tensor_tensor(out=Gim[:], in0=Pre[:, :, 0:32], in1=bc(cim[:].unsqueeze(2), s32), op=ALU.mult), [bp], [bp])
            V(lambda e: e.tensor_tensor(out=g1[:], in0=Pim[:, :, 0:32], in1=bc(cre[:].unsqueeze(2), s32), op=ALU.mult), [bp], [bp])
            V(lambda e: e.tensor_tensor(out=Gim[:], in0=Gim[:], in1=g1[:], op=ALU.add), [bp], [bp])
            S.barrier()
        AR2 = k.sb(st, [128, 32, 2]); AI2 = k.sb(st, [128, 32, 2])
        for h in range(2):
            V(lambda e, h=h: e.tensor_copy(out=AR2[:, :, h], in_=Pre[:, :, 48]), [bp], [bp])
        V(lambda e: e.tensor_scalar(out=AI2[:, :, 0], in0=Pim[:, :, 48], scalar1=-1.0, scalar2=None, op0=ALU.mult), [bp], [bp])
        V(lambda e: e.tensor_copy(out=AI2[:, :, 1], in_=Pim[:, :, 48]), [bp], [bp])
        Tz = k.sb(st, [128, 32, 2])
        Zbf = k.sb(st, [128, 32, 2, NCH + 1], BF16)
        bT, bZbf = Buf("T"), Buf("Zbf")
        with contextlib.ExitStack() as stwz:
            W = k.sb(stwz, [128, 32, 2, NCH])
            bW = Buf("W")
            with contextlib.ExitStack() as st2:
                XB = k.sb(st2, [128, 16, 2, 8, 16])
                x1 = k.sb(st2, [128, 16, 8, 16]); x2 = k.sb(st2, [128, 16, 8, 16])
                XBT = k.sb(st2, [128, 16, 2, 128], BF16)
                uTm = k.sb(st2, [128, 4, 16, NCH], BF16)
                pX = [k.ps(st2) for _ in range(2)]
                pW = [k.ps(st2) for _ in range(4)]
                bxb, bx, bpX, bXBT, bum, bpW = Buf(), Buf(), [Buf(), Buf()], Buf(), Buf(), [Buf() for _ in range(4)]
                s4 = [128, 16, 8, 16]
                n = 0
                nw = 0
                for blk in range(4):
                    gs = slice(blk * 8, blk * 8 + 8)
                    gj = lambda t, gs=gs: bc(t[:, gs, 16:32].rearrange("p g j -> p j g").unsqueeze(3), s4)
                    bb = lambda t, gs=gs: bc(t[:, gs, :].unsqueeze(1), s4)
                    V(lambda e, gj=gj, bb=bb: e.tensor_tensor(out=x1[:], in0=gj(Gre), in1=bb(Bre), op=ALU.mult), [bp, bl], [bx])
                    V(lambda e, gj=gj, bb=bb: e.tensor_tensor(out=x2[:], in0=gj(Gim), in1=bb(Bim), op=ALU.mult), [bp, bl], [bx])
                    V(lambda e: e.tensor_tensor(out=XB[:, :, 0, :, :], in0=x1[:], in1=x2[:], op=ALU.subtract), [bx], [bxb])
                    V(lambda e, gj=gj, bb=bb: e.tensor_tensor(out=x1[:], in0=gj(Gre), in1=bb(Bim), op=ALU.mult), [bp, bl, bxb], [bx])
                    V(lambda e, gj=gj, bb=bb: e.tensor_tensor(out=x2[:], in0=gj(Gim), in1=bb(Bre), op=ALU.mult), [bp, bl], [bx])
                    V(lambda e: e.tensor_tensor(out=XB[:, :, 1, :, :], in0=x1[:], in1=x2[:], op=ALU.add), [bx], [bxb])
                    for j in range(16):
                        sl = n % 2
                        n += 1
                        for h in range(2):
                            T(lambda e, j=j, h=h, sl=sl: e.transpose(
                                out=pX[sl][:, h * 128:(h + 1) * 128], in_=XB[:, j, h, :, :].rearrange("p g h -> p (g h)"),
                                identity=ident_f[:]), [bxb, BP["ident_f"]], [bpX[sl]])
                        A(lambda e, j=j, sl=sl: e.copy(out=XBT[:, j, :, :], in_=pX[sl][:, 0:256].rearrange("p (h n) -> p h n", h=2)),
                          [bpX[sl]], [bXBT])
                    for half8 in range(2):
                        for g4 in range(4):
                            gp = half8 * 4 + g4
                            fn = lambda e, blk=blk, gp=gp, g4=g4: e.tensor_scalar(
                                out=uTm[:, g4, :, :], in0=uT[:, blk, :, :], scalar1=mask8[:, gp:gp + 1], scalar2=None, op0=ALU.mult)
                            (V if gp % 2 == 0 else G)(fn, [BP["uT"], bl], [bum])
                        for g4 in range(4):
                            g = blk * 8 + half8 * 4 + g4
                            for h in range(2):
                                sl = nw % 4
                                nw += 1
                                for j in range(16):
                                    T(lambda e, g4=g4, h=h, j=j, sl=sl: e.matmul(
                                        pW[sl][:, 0:NCH], lhsT=XBT[:, j, h, :], rhs=uTm[:, g4, j, :], start=(j == 0), stop=(j == 15)),
                                      [bXBT, bum], [bpW[sl]])
                                if nw % 2 == 0:
                                    A(lambda e, g=g, h=h, sl=sl: e.copy(out=W[:, g, h, :], in_=pW[sl][:, 0:NCH]), [bpW[sl]], [bW])
                                else:
                                    V(lambda e, g=g, h=h, sl=sl: e.tensor_copy(out=W[:, g, h, :], in_=pW[sl][:, 0:NCH]), [bpW[sl]], [bW])
                S.barrier()
            V(lambda e: e.memset(Tz[:], 0.0), [], [bT])
            if final:
                Fs = k.sb(stwz, [128, 32, 2]); a2r = k.sb(stwz, [128, 32]); a2i = k.sb(stwz, [128, 32])
                AR2s = k.sb(stwz, [128, 32, 2]); AI2s = k.sb(stwz, [128, 32, 2]); h1 = k.sb(stwz, [128, 32, 2]); h2 = k.sb(stwz, [128, 32, 2])
                bF, bh = Buf(), Buf()
                V(lambda e: e.tensor_scalar(out=a2r[:], in0=Pre[:, :, 49], scalar1=-1.0, scalar2=None, op0=ALU.add), [bp], [bh])
                V(lambda e: e.tensor_copy(out=a2i[:], in_=Pim[:, :, 49]), [bp], [bh])
                for s in range(3):
                    S.dma("sp", "F", lambda e, s=s: e.dma_start(out=Fs[0:64], in_=D["F_all"][s, 0:64]), writes=[bF])
                    S.dma("sp", "F", lambda e, s=s: e.dma_start(out=Fs[64:128], in_=D["F_all"][3 - s, 64:128]), writes=[bF])
                    m = mk[:, s:s + 1]
                    for h in range(2):
                        V(lambda e, h=h, m=m: e.tensor_scalar(out=AR2s[:, :, h], in0=a2r[:], scalar1=m, scalar2=1.0, op0=ALU.mult, op1=ALU.add),
                          [bh, bl], [bh])
                    V(lambda e, m=m: e.tensor_scalar(out=AI2s[:, :, 0], in0=a2i[:], scalar1=m, scalar2=-1.0, op0=ALU.mult, op1=ALU.mult),
                      [bh, bl], [bh])
                    V(lambda e, m=m: e.tensor_scalar(out=AI2s[:, :, 1], in0=a2i[:], scalar1=m, scalar2=None, op0=ALU.mult), [bh, bl], [bh])
                    V(lambda e: e.tensor_tensor(out=h1[:], in0=Tz[:], in1=AR2s[:], op=ALU.mult), [bT, bh], [bh])
                    V(lambda e: e.tensor_tensor(out=h2[:, :, 0], in0=Tz[:, :, 1], in1=AI2s[:, :, 0], op=ALU.mult), [bT, bh], [bh])
                    V(lambda e: e.tensor_tensor(out=h2[:, :, 1], in0=Tz[:, :, 0], in1=AI2s[:, :, 1], op=ALU.mult), [bT, bh], [bh])
                    V(lambda e: e.tensor_tensor(out=h1[:], in0=h1[:], in1=h2[:], op=ALU.add), [bh], [bh])
                    V(lambda e, m=m: e.scalar_tensor_tensor(out=Tz[:], in0=Fs[:], scalar=m, in1=h1[:], op0=ALU.mult, op1=ALU.add),
                      [bF, bh, bl], [bT])
            Zu = k.sb(stwz, [128, 32, 2, NCH + 1])
            s1 = k.sb(stwz, [128, 32, 2]); s2 = k.sb(stwz, [128, 32, 2])
            bZ = Buf("Z")
            V(lambda e: e.scalar_tensor_tensor(out=W[0:64, :, :, 0], in0=W[0:64, :, :, 0], scalar=mk[0:64, 3:4], in1=Tz[0:64],
                                               op0=ALU.mult, op1=ALU.add), [bW, bT, bl], [bW])
            V(lambda e: e.memset(Zu[0:64, :, :, 0], 0.0), [], [bZ])
            G(lambda e: e.tensor_copy(out=Zu[64:128, :, :, NCH - 1], in_=Tz[64:128]), [bT], [bZ])
            bsf, bsb, bZf, bZb = Buf(), Buf(), Buf(), Buf()
            S.barrier()
            for c in range(NCH):
                r = slice(0, 64)
                V(lambda e, c=c, r=r: e.tensor_tensor(out=s1[r], in0=Zu[r, :, :, c], in1=AR2[r], op=ALU.mult), [bZf], [bsf])
                V(lambda e, c=c, r=r: e.tensor_tensor(out=s2[r, :, 0], in0=Zu[r, :, 1, c], in1=AI2[r, :, 0], op=ALU.mult), [bZf], [bsf])
                V(lambda e, c=c, r=r: e.tensor_tensor(out=s2[r, :, 1], in0=Zu[r, :, 0, c], in1=AI2[r, :, 1], op=ALU.mult), [bZf], [bsf])
                V(lambda e, c=c, r=r: e.tensor_tensor(out=s1[r], in0=s1[r], in1=s2[r], op=ALU.add), [bsf], [bsf])
                V(lambda e, c=c, r=r: e.tensor_tensor(out=Zu[r, :, :, c + 1], in0=s1[r], in1=W[r, :, :, c], op=ALU.add), [bsf], [bZf])
                cb = NCH - 1 - c
                if cb >= 1:
                    r = slice(64, 128)
                    G(lambda e, cb=cb, r=r: e.tensor_tensor(out=s1[r], in0=Zu[r, :, :, cb], in1=AR2[r], op=ALU.mult), [bZb], [bsb])
                    G(lambda e, cb=cb, r=r: e.tensor_tensor(out=s2[r, :, 0], in0=Zu[r, :, 1, cb], in1=AI2[r, :, 0], op=ALU.mult), [bZb], [bsb])
                    G(lambda e, cb=cb, r=r: e.tensor_tensor(out=s2[r, :, 1], in0=Zu[r, :, 0, cb], in1=AI2[r, :, 1], op=ALU.mult), [bZb], [bsb])
                    G(lambda e, cb=cb, r=r: e.tensor_tensor(out=s1[r], in0=s1[r], in1=s2[r], op=ALU.add), [bsb], [bsb])
                    G(lambda e, cb=cb, r=r: e.tensor_tensor(out=Zu[r, :, :, cb - 1], in0=s1[r], in1=W[r, :, :, cb], op=ALU.add), [bsb], [bZb])
            S.barrier()
            if not final:
                bo = Buf()
                V(lambda e: e.tensor_copy(out=Tz[0:64], in_=Zu[0:64, :, :, NCH]), [bZf], [bT])
                V(lambda e: e.tensor_copy(out=Tz[64:128], in_=Zu[64:128, :, :, 0]), [bZb], [bT])
                S.dma("sp", "o", lambda e: e.dma_start(out=k.dout["F"], in_=Tz[:]), reads=[bT], writes=[bo])
                S.barrier()
                return
            A(lambda e: e.copy(out=Zbf[:], in_=Zu[:]), [bZf, bZb], [bZbf])
            S.barrier()
        Kmat = k.sb(st, [128, 4, 32, 128], BF16)
        Dmat = k.sb(st, [128, 4, 128], BF16)
        Yc = k.sb(st, [128, 32, 16, 2, 16], BF16)
        bK, bY = Buf(), Buf()
        for blk in range(4):
            V(lambda e, blk=blk: e.tensor_scalar(out=Dmat[:, blk, :], in0=ident_f[:], scalar1=dvec[:, blk:blk + 1], scalar2=None, op0=ALU.mult),
              [BP["ident_f"], bl], [bK])
        with contextlib.ExitStack() as st2:
            RC = k.sb(st2, [128, 8, 16, 16], BF16); NI = k.sb(st2, [128, 8, 16, 16], BF16)
            c1 = k.sb(st2, [128, 8, 16, 16]); c2 = k.sb(st2, [128, 8, 16, 16])
            Bpr = k.sb(st2, [128, 8, 128], BF16); Bpi = k.sb(st2, [128, 8, 128], BF16)
            pK = [k.ps(st2) for _ in range(2)]
            bc1, bRC, bBp, bpK = Buf(), Buf(), Buf(), [Buf(), Buf()]
            s5 = [128, 8, 16, 16]
            n = 0
            for blk in range(4):
                gs = slice(blk * 8, blk * 8 + 8)
                cb_ = lambda t, gs=gs: bc(t[:, gs, :].unsqueeze(2), s5)
                gd = lambda t, lo, gs=gs: bc(t[:, gs, lo:lo + 16].unsqueeze(3), s5)
                V(lambda e, cb_=cb_, gd=gd: e.tensor_tensor(out=c1[:], in0=cb_(Cre), in1=gd(Gre, 0), op=ALU.mult), [bl, bp, bRC], [bc1])
                V(lambda e, cb_=cb_, gd=gd: e.tensor_tensor(out=c2[:], in0=cb_(Cim), in1=gd(Gim, 0), op=ALU.mult), [bl, bp], [bc1])
                V(lambda e: e.tensor_tensor(out=RC[:], in0=c1[:], in1=c2[:], op=ALU.subtract), [bc1], [bRC])
                V(lambda e, cb_=cb_, gd=gd: e.tensor_tensor(out=c1[:], in0=cb_(Cre), in1=gd(Gim, 0), op=ALU.mult), [bl, bp, bRC], [bc1])
                V(lambda e, cb_=cb_, gd=gd: e.tensor_tensor(out=c2[:], in0=cb_(Cim), in1=gd(Gre, 0), op=ALU.mult), [bl, bp], [bc1])
                V(lambda e: e.tensor_tensor(out=c1[:], in0=c1[:], in1=c2[:], op=ALU.add), [bc1], [bc1])
                V(lambda e: e.tensor_scalar(out=NI[:], in0=c1[:], scalar1=-1.0, scalar2=None, op0=ALU.mult), [bc1], [bRC])
                V(lambda e, cb_=cb_, gd=gd: e.tensor_tensor(out=c1[:], in0=cb_(Cre), in1=gd(Pre, 32), op=ALU.mult), [bl, bp, bRC], [bc1])
                V(lambda e, cb_=cb_, gd=gd: e.tensor_tensor(out=c2[:], in0=cb_(Cim), in1=gd(Pim, 32), op=ALU.mult), [bl, bp], [bc1])
                V(lambda e, gs=gs: e.tensor_tensor(out=Yc[:, gs, :, 0, :], in0=c1[:], in1=c2[:], op=ALU.subtract), [bc1], [bY])
                V(lambda e, cb_=cb_, gd=gd: e.tensor_tensor(out=c1[:], in0=cb_(Cre), in1=gd(Pim, 32), op=ALU.mult), [bl, bp, bY], [bc1])
                V(lambda e, cb_=cb_, gd=gd: e.tensor_tensor(out=c2[:], in0=cb_(Cim), in1=gd(Pre, 32), op=ALU.mult), [bl, bp], [bc1])
                V(lambda e: e.tensor_tensor(out=c1[:], in0=c1[:], in1=c2[:], op=ALU.add), [bc1], [bc1])
                V(lambda e, gs=gs: e.tensor_scalar(out=Yc[:, gs, :, 1, :], in0=c1[:], scalar1=-1.0, scalar2=None, op0=ALU.mult), [bc1], [bY])
                G(lambda e: e.memset(Bpr[:], 0.0), [], [bBp])
                G(lambda e: e.memset(Bpi[:], 0.0), [], [bBp])
                for gp in range(8):
                    G(lambda e, gp=gp, blk=blk: e.tensor_copy(out=Bpr[:, gp, gp * 16:(gp + 1) * 16], in_=Bre[:, blk * 8 + gp, :]), [bl], [bBp])
                    G(lambda e, gp=gp, blk=blk: e.tensor_copy(out=Bpi[:, gp, gp * 16:(gp + 1) * 16], in_=Bim[:, blk * 8 + gp, :]), [bl], [bBp])
                for d in range(2):
                    r = slice(d * 64, d * 64 + 64)
                    for qd in range(4):
                        sl = n % 2
                        n += 1
                        for gp in range(8):
                            oap = lambda sl=sl, gp=gp: pK[sl][:].rearrange("p (dl c) -> p dl c", dl=4)[:, :, gp * 16:(gp + 1) * 16]
                            T(lambda e, r=r, gp=gp, qd=qd, oap=oap: e.matmul(oap(), lhsT=Bpr[r, gp, :], rhs=RC[r, gp, qd * 4:qd * 4 + 4, :],
                                                                            start=True, stop=False), [bBp, bRC], [bpK[sl]])
                            T(lambda e, r=r, gp=gp, qd=qd, oap=oap: e.matmul(oap(), lhsT=Bpi[r, gp, :], rhs=NI[r, gp, qd * 4:qd * 4 + 4, :],
                                                                            start=False, stop=True), [bBp, bRC], [bpK[sl]])
                        t0 = d * 16 + qd * 4
                        A(lambda e, blk=blk, t0=t0, sl=sl: e.copy(out=Kmat[:, blk, t0:t0 + 4, :], in_=pK[sl][:].rearrange("p (dl c) -> p dl c", dl=4)),
                          [bpK[sl]], [bK])
            S.barrier()
        with contextlib.ExitStack() as st2:
            ysb = k.sb(st2, [128, 16, 128], BF16)
            ygb = k.sb(st2, [128, 4, 16, 128], BF16)
            sqx = k.sb(st2, [128, 512]); tt = k.sb(st2, [128, 512]); sg = k.sb(st2, [128, 512])
            wg_st = k.sb(st2, [128, 512]); wg = k.sb(st2, [128, 4, 512], BF16); bg = k.sb(st2, [128, 4])
            pYs = [k.ps(st2) for _ in range(2)]
            pY = [k.ps(st2) for _ in range(4)]
            pG = k.ps(st2)
            bys, byg, bge, bwg, bws, bpYs, bpY, bpG = Buf(), Buf(), Buf(), Buf(), Buf(), [Buf(), Buf()], [Buf() for _ in range(4)], Buf()
            for kb in range(4):
                S.dma("sp", "wg", lambda e, kb=kb: e.dma_start(out=wg_st[:], in_=D["w_glu"][kb * 128:(kb + 1) * 128, :]), writes=[bws])
                A(lambda e, kb=kb: e.copy(out=wg[:, kb, :], in_=wg_st[:]), [bws], [bwg])
            S.dma("sp", "c", lambda e: e.dma_start(out=bg[:], in_=D["s_bglu"]), writes=[bwg])
            for blk in range(4):
                for ih in range(4):
                    sl = ih % 2
                    for il in range(4):
                        i = ih * 4 + il
                        for gp in range(8):
                            g = blk * 8 + gp
                            for h in range(2):
                                T(lambda e, g=g, h=h, i=i, il=il, gp=gp, sl=sl: e.matmul(
                                    pYs[sl][:, il * 128 + gp * 16: il * 128 + gp * 16 + 16], lhsT=Zbf[:, g, h, 1:129], rhs=Yc[:, g, i, h, :],
                                    start=(h == 0), stop=(h == 1)), [bZbf, bY], [bpYs[sl]])
                    A(lambda e, ih=ih, sl=sl: e.copy(out=ysb[:, ih * 4:ih * 4 + 4, :], in_=pYs[sl][:].rearrange("p (i c) -> p i c", i=4)),
                      [bpYs[sl]], [bys])
                for q in range(4):
                    i0 = 4 * q
                    rr = [bK, BP["uT"]]
                    ww = [bpY[q]]
                    T(lambda e, blk=blk, q=q, i0=i0: e.matmul(pY[q][:], lhsT=Kmat[:, blk, 0, :], rhs=uT[:, blk, i0:i0 + 4, 1:129],
                                                             start=True, stop=False), rr, ww)
                    T(lambda e, blk=blk, q=q, i0=i0: e.matmul(pY[q][:], lhsT=Kmat[:, blk, 16, :], rhs=uT[:, blk, i0:i0 + 4, 1:129],
                                                             start=False, stop=False), rr, ww)
                    T(lambda e, blk=blk, q=q, i0=i0: e.matmul(pY[q][:], lhsT=Dmat[:, blk, :], rhs=uT[:, blk, i0:i0 + 4, 1:129],
                                                             start=False, stop=False), rr, ww)
                    for dl in range(1, 16):
                        lo, hi = max(i0, dl), i0 + 4
                        if lo < hi:
                            T(lambda e, blk=blk, q=q, i0=i0, lo=lo, hi=hi, dl=dl: e.matmul(
                                pY[q][:, (lo - i0) * 128:512], lhsT=Kmat[:, blk, dl, :], rhs=uT[:, blk, lo - dl:hi - dl, 1:129],
                                start=False, stop=False), rr, ww)
                        lo, hi = i0, min(i0 + 4, 16 - dl)
                        if lo < hi:
                            T(lambda e, blk=blk, q=q, i0=i0, lo=lo, hi=hi, dl=dl: e.matmul(
                                pY[q][:, 0:(hi - i0) * 128], lhsT=Kmat[:, blk, 16 + dl, :], rhs=uT[:, blk, lo + dl:hi + dl, 1:129],
                                start=False, stop=False), rr, ww)
                    for il in range(4):
                        T(lambda e, q=q, il=il, i0=i0: e.matmul(pY[q][:, il * 128:(il + 1) * 128], lhsT=ysb[:, i0 + il, :], rhs=ident_b[:],
                                                               start=False, stop=(il == 3)), [bys, BP["ident_b"]], ww)
                    A(lambda e, q=q: e.activation(out=sqx[:], in_=pY[q][:], func=AF.Square), [bpY[q]], [bge])
                    V(lambda e: e.tensor_scalar(out=tt[:], in0=sqx[:], scalar1=0.044715, scalar2=1.0, op0=ALU.mult, op1=ALU.add), [bge], [bge])
                    V(lambda e, q=q: e.tensor_tensor(out=tt[:], in0=tt[:], in1=pY[q][:], op=ALU.mult), [bge, bpY[q]], [bge])
                    A(lambda e: e.activation(out=sg[:], in_=tt[:], func=AF.Sigmoid, scale=1.5957691216057308), [bge], [bge])
                    V(lambda e, q=q, blk=blk, i0=i0: e.tensor_tensor(out=ygb[:, blk, i0:i0 + 4, :].rearrange("p i c -> p (i c)"), in0=sg[:],
                                                                    in1=pY[q][:], op=ALU.mult), [bge, bpY[q]], [byg])
            for ob in range(4):
                for tq_ in range(4):
                    for kb in range(4):
                        T(lambda e, ob=ob, tq_=tq_, kb=kb: e.matmul(pG[:], lhsT=wg[:, kb, ob * 128:(ob + 1) * 128],
                                                                    rhs=ygb[:, kb, tq_ * 4:tq_ * 4 + 4, :], start=(kb == 0), stop=(kb == 3)),
                          [bwg, byg], [bpG])
                    A(lambda e, ob=ob: e.activation(out=sg[:], in_=pG[:], func=AF.Sigmoid, bias=bg[:, ob:ob + 1], scale=1.0), [bpG, bwg], [bge])
                    V(lambda e, ob=ob, tq_=tq_: e.tensor_tensor(
                        out=P["ysT"][:, ob, :].rearrange("p (c i) -> p i c", i=16)[:, tq_ * 4:tq_ * 4 + 4, :],
                        in0=sg[:].rearrange("p (i c) -> p i c", i=4), in1=ygb[:, ob, tq_ * 4:tq_ * 4 + 4, :], op=ALU.mult),
                      [bge, byg], [BP["ysT"]])
            S.barrier()


def ln_tile(S, k, x, stt, mv, rstd, g_t, b_t, bx, bstat, bgb, eps=1e-5):
    for c in range(2):
        S.op("dve", lambda e, c=c: e.bn_stats(out=stt[:, c, :], in_=x[:, c * 512:(c + 1) * 512]), reads=[bx], writes=[bstat])
    S.op("dve", lambda e: e.bn_aggr(out=mv[:], in_=stt[:]), reads=[bstat], writes=[bstat])
    S.op("act", lambda e: e.activation(out=rstd[:], in_=mv[:, 1:2], func=AF.Sqrt, bias=eps, scale=1.0), reads=[bstat], writes=[bstat])
    S.op("dve", lambda e: e.reciprocal(out=rstd[:], in_=rstd[:]), reads=[bstat], writes=[bstat])
    S.op("dve", lambda e: e.tensor_scalar(out=x[:], in0=x[:], scalar1=mv[:, 0:1], scalar2=rstd[:, 0:1], op0=ALU.subtract, op1=ALU.mult),
         reads=[bx, bstat], writes=[bx])
    S.op("pool", lambda e: e.tensor_tensor(out=x[:], in0=x[:], in1=g_t[:], op=ALU.mult), reads=[bx, bgb], writes=[bx])
    S.op("pool", lambda e: e.tensor_tensor(out=x[:], in0=x[:], in1=b_t[:], op=ALU.add), reads=[bx, bgb], writes=[bx])


def merge(k, P):
    nc, S = k.nc, k.S
    D = k.din
    BP = P["B"]
    ysT, ident_f = P["ysT"], P["ident_f"]
    V = lambda fn, r, w: S.op("dve", fn, reads=r, writes=w)
    A = lambda fn, r, w: S.op("act", fn, reads=r, writes=w)
    G = lambda fn, r, w: S.op("pool", fn, reads=r, writes=w)
    T = lambda fn, r, w: S.op("pe", fn, reads=r, writes=w)
    with contextlib.ExitStack() as st:
        yab = [k.sb(st, [128, 4, 512], BF16) for _ in range(2)]
        wgt = k.sb(st, [128, 8, 2048], BF16)
        wab = k.sb(st, [128, 4, 1024], BF16)
        wsb = k.sb(st, [128, 4, 1024], BF16)
        wo = k.sb(st, [128, 8, 1024], BF16)
        wr = k.sb(st, [128, 8, 16])
        stg = [k.sb(st, [128, 2048]) for _ in range(2)]
        g_t = k.sb(st, [128, 1024]); b_t = k.sb(st, [128, 1024])
        hTb = [k.sb(st, [128, 8, 512], BF16) for _ in range(2)]
        sga = k.sb(st, [128, 512]); sgs = k.sb(st, [128, 512]); m1 = k.sb(st, [128, 512])
        mT = k.sb(st, [128, 8, 512], BF16)
        h0t = [k.sb(st, [128, 1024]) for _ in range(2)]
        x1 = [k.sb(st, [128, 1024]) for _ in range(2)]
        h1T = k.sb(st, [128, 8, 128])
        stt = k.sb(st, [128, 2, 6]); mv = k.sb(st, [128, 2]); rstd = k.sb(st, [128, 1])
        lg = k.sb(st, [128, 16]); mx = k.sb(st, [128, 1]); sm = k.sb(st, [128, 1]); af = [k.sb(st, [128, 16]) for _ in range(2)]
        pGa, pBa, pGs, pBs, pO, pL = [k.ps(st) for _ in range(6)]
        pT = [k.ps(st) for _ in range(2)]
        bw, bstg, bgb, bhT, bsg, bm1, bmT, bh0, bx1, bh1T, bstat, blg, baf = (Buf(), [Buf(), Buf()], Buf(), [Buf(), Buf()], Buf(), Buf(), Buf(),
                                                                            [Buf(), Buf()], [Buf(), Buf()], Buf(), Buf(), Buf(), [Buf(), Buf()])
        bpGa, bpBa, bpGs, bpBs, bpO, bpL, bpT, bo = Buf(), Buf(), Buf(), Buf(), Buf(), Buf(), [Buf(), Buf()], Buf()
        S.dma("sp", "c", lambda e: e.dma_start(out=g_t[:], in_=D["ln1_g"].partition_broadcast(128)), writes=[bgb])
        S.dma("sp", "c", lambda e: e.dma_start(out=b_t[:], in_=D["ln1_b"].partition_broadcast(128)), writes=[bgb])
        S.dma("sp", "c", lambda e: e.dma_start(out=wr[:], in_=D["w_router"].rearrange("(kb p) n -> p kb n", p=128)), writes=[bw])
        n = 0
        loads = [(wgt, kb, "w_gates", 2048) for kb in range(8)] + [(wab, kb, "w_attn_br", 1024) for kb in range(4)] + \
                [(wsb, kb, "w_ssm_br", 1024) for kb in range(4)] + [(wo, kb, "w_o", 1024) for kb in range(8)]
        for dst, kb, nm, wd_ in loads:
            sl = n % 2
            n += 1
            S.dma("sp", f"mst{sl}", lambda e, sl=sl, kb=kb, nm=nm, wd_=wd_: e.dma_start(out=stg[sl][:, 0:wd_], in_=D[nm][kb * 128:(kb + 1) * 128, :]),
                  writes=[bstg[sl]])
            (A if n % 2 == 0 else G)(lambda e, sl=sl, kb=kb, dst=dst, wd_=wd_: (e.copy if hasattr(e, "copy") else e.tensor_copy)(
                out=dst[:, kb, :], in_=stg[sl][:, 0:wd_]), [bstg[sl]], [bw])
        for tb in range(4):
            sl = tb % 2
            t0 = tb * 512
            S.dma("sp", f"mh{sl}", lambda e, sl=sl, t0=t0: e.dma_start(out=hTb[sl][:], in_=P["hT_d"][:, :, 128 + t0:128 + t0 + 512]),
                  reads=[BP["hT_d"]], writes=[bhT[sl]])
            S.dma("sp", f"mh{sl}", lambda e, sl=sl, t0=t0: e.dma_start(out=yab[sl][:], in_=P["yaT_d"][:, :, t0:t0 + 512]),
                  reads=[BP["yaT_d"]], writes=[bhT[sl]])
            for ft in range(8):
                fs = slice(ft * 128, (ft + 1) * 128)
                fs2 = slice(1024 + ft * 128, 1024 + (ft + 1) * 128)
                for kb in range(8):
                    T(lambda e, kb=kb, fs=fs, sl=sl: e.matmul(pGa[:], lhsT=wgt[:, kb, fs], rhs=hTb[sl][:, kb, :], start=(kb == 0), stop=(kb == 7)),
                      [bw, bhT[sl]], [bpGa])
                for kb in range(8):
                    T(lambda e, kb=kb, fs2=fs2, sl=sl: e.matmul(pGs[:], lhsT=wgt[:, kb, fs2], rhs=hTb[sl][:, kb, :], start=(kb == 0), stop=(kb == 7)),
                      [bw, bhT[sl]], [bpGs])
                for kb in range(4):
                    T(lambda e, kb=kb, fs=fs, sl=sl: e.matmul(pBa[:], lhsT=wab[:, kb, fs], rhs=yab[sl][:, kb, :], start=(kb == 0), stop=(kb == 3)),
                      [bw, bhT[sl]], [bpBa])
                for kb in range(4):
                    T(lambda e, kb=kb, fs=fs, t0=t0: e.matmul(pBs[:], lhsT=wsb[:, kb, fs], rhs=ysT[:, kb, t0:t0 + 512], start=(kb == 0), stop=(kb == 3)),
                      [bw, BP["ysT"]], [bpBs])
                A(lambda e: e.activation(out=sga[:], in_=pGa[:], func=AF.Sigmoid), [bpGa], [bsg])
                A(lambda e: e.activation(out=sgs[:], in_=pGs[:], func=AF.Sigmoid), [bpGs], [bsg])
                V(lambda e: e.tensor_tensor(out=m1[:], in0=sga[:], in1=pBa[:], op=ALU.mult), [bsg, bpBa], [bm1])
                V(lambda e: e.tensor_tensor(out=sgs[:], in0=sgs[:], in1=pBs[:], op=ALU.mult), [bsg, bpBs], [bsg])
                V(lambda e, ft=ft: e.tensor_tensor(out=mT[:, ft, :], in0=m1[:], in1=sgs[:], op=ALU.add), [bm1, bsg], [bmT])
            for tt in range(4):
                ti_ = tb * 4 + tt
                s2 = ti_ % 2
                S.dma("sp", f"mh0{s2}", lambda e, s2=s2, ti_=ti_: e.dma_start(out=h0t[s2][:], in_=P["h0_d"][ti_ * 128:(ti_ + 1) * 128, :]),
                      reads=[BP["h0_d"]], writes=[bh0[s2]])
                for dh in range(2):
                    for kb in range(8):
                        T(lambda e, kb=kb, tt=tt, dh=dh: e.matmul(pO[:], lhsT=mT[:, kb, tt * 128:(tt + 1) * 128], rhs=wo[:, kb, dh * 512:(dh + 1) * 512],
                                                                 start=(kb == 0), stop=(kb == 7)), [bmT, bw], [bpO])
                    V(lambda e, s2=s2, dh=dh: e.scalar_tensor_tensor(out=x1[s2][:, dh * 512:(dh + 1) * 512], in0=h0t[s2][:, dh * 512:(dh + 1) * 512],
                                                                    scalar=ALPHA, in1=pO[:], op0=ALU.mult, op1=ALU.add), [bh0[s2], bpO], [bx1[s2]])
                ln_tile(S, k, x1[s2], stt, mv, rstd, g_t, b_t, bx1[s2], bstat, bgb)
                S.dma("sp", f"mo{s2}", lambda e, s2=s2, ti_=ti_: e.dma_start(out=k.dout["h1"][ti_ * 128:(ti_ + 1) * 128, :], in_=x1[s2][:]),
                      reads=[bx1[s2]], writes=[bo])
                for kb in range(8):
                    T(lambda e, kb=kb, s2=s2: e.transpose(out=pT[kb // 4][:, (kb % 4) * 128:(kb % 4 + 1) * 128], in_=x1[s2][:, kb * 128:(kb + 1) * 128],
                                                         identity=ident_f[:]), [bx1[s2], BP["ident_f"]], [bpT[kb // 4]])
                for hh in range(2):
                    A(lambda e, hh=hh: e.copy(out=h1T[:, hh * 4:(hh + 1) * 4, :], in_=pT[hh][:].rearrange("p (a n) -> p a n", a=4)), [bpT[hh]], [bh1T])
                for kb in range(8):
                    T(lambda e, kb=kb: e.matmul(pL[:, 0:16], lhsT=h1T[:, kb, :], rhs=wr[:, kb, :], start=(kb == 0), stop=(kb == 7)), [bh1T, bw], [bpL])
                V(lambda e: e.tensor_copy(out=lg[:], in_=pL[:, 0:16]), [bpL], [blg])
                V(lambda e: e.tensor_reduce(out=mx[:], in_=lg[:], axis=AX.X, op=ALU.max), [blg], [blg])
                V(lambda e: e.tensor_scalar(out=mx[:], in0=mx[:], scalar1=-1.0, scalar2=None, op0=ALU.mult), [blg], [blg])
                A(lambda e: e.activation(out=lg[:], in_=lg[:], func=AF.Exp, bias=mx[:, 0:1], scale=1.0), [blg], [blg])
                V(lambda e: e.tensor_reduce(out=sm[:], in_=lg[:], axis=AX.X, op=ALU.add), [blg], [blg])
                V(lambda e: e.reciprocal(out=sm[:], in_=sm[:]), [blg], [blg])
                V(lambda e, s2=s2: e.tensor_scalar(out=af[s2][:], in0=lg[:], scalar1=sm[:, 0:1], scalar2=None, op0=ALU.mult), [blg], [baf[s2]])
                S.dma("sp", f"ma{s2}", lambda e, s2=s2, ti_=ti_: e.dma_start(out=k.dout["aff"][ti_ * 128:(ti_ + 1) * 128, :], in_=af[s2][:]),
                      reads=[baf[s2]], writes=[bo])
        S.barrier()

def alloc_persist(k, st):
    P = {}
    P["ident_b"] = k.sb(st, [128, 128], BF16, "identb")
    P["ident_f"] = k.sb(st, [128, 128], F32, "identf")
    P["ysT"] = k.sb(st, [128, 4, TOK], BF16, "ysT")
    P["hT_d"] = k.nc.dram_tensor("hT_d", [128, 8, NT * 128], BF16).ap()
    P["h0_d"] = k.nc.dram_tensor("h0_d", [TOK, 1024], F32).ap()
    P["yaT_d"] = k.nc.dram_tensor("yaT_d", [128, 4, TOK], BF16).ap()
    P["B"] = {n: Buf(n) for n in ["ident_b", "hT_d", "h0_d", "yaT_d", "qT", "kTo", "vo", "uT", "ysT", "ident_f"]}
    return P


def alloc_stage(k, st, P):
    P["qT"] = k.sb(st, [128, 4, NT * 128], BF16, "qT")
    P["kTo"] = k.sb(st, [128, NT * 128], BF16, "kTo")
    P["vo"] = k.sb(st, [128, NT, 2, 65], BF16, "vo")
    P["uT"] = k.sb(st, [128, 4, 16, NCH], BF16, "uT")


def build(stage):
    k = K()
    nc, S = k.nc, k.S
    k.inp("ident", [128, 128])
    k.inp("xin", [NT * 128, 1024])
    k.inp("ln_in_g", [1024])
    k.inp("ln_in_b", [1024])
    k.inp("qkg", [640])
    k.inp("w_qkvu", [1024, 1280])
    k.inp("rope", [NT * 128, 2, 32])
    want_ssm = stage in ("L1", "dbg_ssm", "L2")
    if want_ssm:
        for nm, shp in [("s_are", [128, 32]), ("s_aim", [128, 32]), ("s_ldt", [128, 32]), ("s_bre", [128, 32, 16]), ("s_bim", [128, 32, 16]),
                        ("s_cre", [128, 32, 16]), ("s_cim", [128, 32, 16]), ("s_eall", [128, NE]), ("s_mask8", [128, 8]),
                        ("s_dvec", [128, 4]), ("s_mk", [128, 4]), ("s_bglu", [128, 4]), ("w_glu", [512, 512])]:
            k.inp(nm, shp)
    if stage in ("dbg_ssm", "L2"):
        k.inp("F_all", [4, 128, 32, 2])
    if stage in ("dbg_attn", "L2"):
        k.inp("kT_all", [128, 65 * 128], BF16)
        k.inp("v_all", [128, 65, 2, 65], BF16)
    if stage == "L2":
        for nm, shp in [("w_gates", [1024, 2048]), ("w_attn_br", [512, 1024]), ("w_ssm_br", [512, 1024]), ("w_o", [1024, 1024]),
                        ("w_router", [1024, 16]), ("ln1_g", [1024]), ("ln1_b", [1024])]:
            k.inp(nm, shp)
        k.out("h1", [TOK, 1024])
        k.out("aff", [TOK, 16])
    if stage in ("L1", "dbg_s1"):
        k.out("d_kT", [128, NT * 128], BF16)
        k.out("d_v", [128, NT, 2, 65], BF16)
    if stage == "L1":
        k.out("F", [128, 32, 2])
    if stage == "dbg_s1":
        k.out("d_qT", [128, 4, NT * 128], BF16)
        k.out("d_uT", [128, 4, 16, NCH], BF16)
    if stage == "dbg_attn":
        k.out("d_yaT", [128, 4, TOK], BF16)
    if stage == "dbg_ssm":
        k.out("d_ysT", [128, 4, TOK], BF16)
    with k.top:
        st = k.top
        P = alloc_persist(k, st)
        BP = P["B"]
        bo = Buf("out")
        with contextlib.ExitStack() as stx:
            alloc_stage(k, stx, P)
            S.op("pool", lambda e: e.memset(P["vo"][:], 1.0), writes=[BP["vo"]])
            stage1(k, P, want_h0=True)
            od = lambda name, t, b: S.dma("sp", "o", lambda e: e.dma_start(out=k.dout[name], in_=t[:]), reads=[b], writes=[bo])
            if stage in ("L1", "dbg_s1"):
                od("d_kT", P["kTo"], BP["kTo"])
                od("d_v", P["vo"], BP["vo"])
            if stage == "dbg_s1":
                od("d_qT", P["qT"], BP["qT"])
                od("d_uT", P["uT"], BP["uT"])
            if stage == "L1":
                ssm(k, P, final=False)
            if stage in ("dbg_ssm", "L2"):
                ssm(k, P, final=True)
            if stage == "dbg_ssm":
                od("d_ysT", P["ysT"], BP["ysT"])
            if stage in ("dbg_attn", "L2"):
                attention(k, P)
            S.barrier()
        if stage == "L2":
            merge(k, P)
        S.barrier()
        with nc.Block() as block:
            S.emit(block)
    return k


QPERM = [0, 4, 1, 5, 2, 6, 3, 7]


def rope_tables():
    half = 32
    inv = (10000.0 ** (-np.arange(0, half, 2, dtype=np.float32) / half)).astype(np.float32)
    s = np.arange(8192)
    row = (s // 64).astype(np.float32)
    col = (s % 64).astype(np.float32)
    ang = np.concatenate([row[:, None] * inv, col[:, None] * inv], axis=-1).astype(np.float32)
    return np.cos(ang).astype(np.float32), np.sin(ang).astype(np.float32)


def common_inputs(inputs, c):
    b, r = c // 4, c % 4
    x = inputs["x"]
    xin = np.zeros((NT * 128, 1024), np.float32)
    xin[0:16] = inputs["meta_tokens"]
    xin[128:] = x[b, r * TOK:(r + 1) * TOK]
    cos, sin = rope_tables()
    rope = np.zeros((NT * 128, 2, 32), np.float32)
    rope[:128, 0] = 1.0
    rope[128:, 0] = cos[r * TOK:(r + 1) * TOK]
    rope[128:, 1] = sin[r * TOK:(r + 1) * TOK]
    w_in = inputs["w_in"][0]
    qcols = np.concatenate([np.arange(h * 64, (h + 1) * 64) for h in QPERM])
    w_qkvu = np.ascontiguousarray(np.concatenate([w_in[:, qcols], w_in[:, 512:1280]], axis=1))
    qkg = np.concatenate([np.tile(inputs["q_norm_g"][0], 8), np.tile(inputs["k_norm_g"][0], 2)]).astype(np.float32)
    return {
        "ident": np.eye(128, dtype=np.float32), "xin": xin, "ln_in_g": inputs["ln_in_g"], "ln_in_b": inputs["ln_in_b"],
        "qkg": qkg, "w_qkvu": w_qkvu, "rope": rope,
    }


def assemble_kv(res1, c):
    b = c // 4
    kT = np.zeros((128, 65 * 128), NPBF)
    v = np.zeros((128, 65, 2, 65), NPBF)
    for r in range(4):
        o = res1[b * 4 + r]
        kT[:, r * 2048:(r + 1) * 2048] = o["d_kT"][:, 128:]
        v[:, r * 16:(r + 1) * 16] = o["d_v"][:, 1:]
    kT[:, 8192:8192 + 128] = res1[b * 4]["d_kT"][:, 0:128]
    v[:, 64] = res1[b * 4]["d_v"][:, 0]
    return kT, v


def ssm_inputs(inputs, c):
    r = c % 4
    f = lambda a: np.ascontiguousarray(a, dtype=np.float32)
    o = {}
    o["s_are"] = f(inputs["ssm_a_re"][0].transpose(0, 2, 1).reshape(128, 32))
    o["s_aim"] = f(inputs["ssm_a_im"][0].transpose(0, 2, 1).reshape(128, 32))
    o["s_ldt"] = f(np.repeat(inputs["ssm_log_dt"][0][:, None, :], 64, axis=1).reshape(128, 32))
    o["s_bre"] = f(inputs["ssm_b_re"][0].transpose(0, 2, 1, 3).reshape(128, 32, 16))
    o["s_bim"] = f(inputs["ssm_b_im"][0].transpose(0, 2, 1, 3).reshape(128, 32, 16))
    o["s_cre"] = f(inputs["ssm_c_re"][0].transpose(0, 3, 1, 2).reshape(128, 32, 16))
    o["s_cim"] = f(inputs["ssm_c_im"][0].transpose(0, 3, 1, 2).reshape(128, 32, 16))
    e = np.zeros((128, NE), np.float32)
    ar = np.arange(16, dtype=np.float32)
    e[:, 0:16] = ar
    e[0:64, 16:32] = 15 - ar
    e[64:, 16:32] = ar
    e[0:64, 32:48] = ar + 1
    e[64:, 32:48] = 16 - ar
    e[:, 48] = 16
    e[:, 49] = 2048
    e[:, 50] = 1
    o["s_eall"] = e
    m8 = np.zeros((128, 8), np.float32)
    for g in range(8):
        m8[g * 16:(g + 1) * 16, g] = 1
    o["s_mask8"] = m8
    o["s_dvec"] = f(inputs["ssm_d"][0].reshape(4, 128).T)
    mk = np.zeros((128, 4), np.float32)
    for s_ in range(3):
        mk[0:64, s_] = 1.0 if s_ < r else 0.0
        mk[64:, s_] = 1.0 if (3 - s_) > r else 0.0
    mk[0:64, 3] = 1.0 if r == 0 else 0.0
    o["s_mk"] = mk
    o["s_bglu"] = f(inputs["b_glu"][0].reshape(4, 128).T)
    o["w_glu"] = f(inputs["w_glu"][0])
    return o


def l2_inputs(inputs, maps1, res1):
    w_in = inputs["w_in"][0]
    out = []
    for c in range(8):
        b = c // 4
        kT, v = assemble_kv(res1, c)
        m = dict(maps1[c])
        m["kT_all"] = kT
        m["v_all"] = v
        m["F_all"] = np.stack([res1[b * 4 + r]["F"] for r in range(4)])
        m["w_gates"] = np.ascontiguousarray(w_in[:, 1280:3328])
        m["w_attn_br"] = inputs["w_attn_br"][0]
        m["w_ssm_br"] = inputs["w_ssm_br"][0]
        m["w_o"] = inputs["w_o"][0]
        m["w_router"] = inputs["w_router"][0]
        m["ln1_g"] = inputs["ln1_g"][0]
        m["ln1_b"] = inputs["ln1_b"][0]
        out.append(m)
    return out

NBIS = 30


def build_l3(debug=False, nexp=16, unit_gate=False):
    k = K()
    nc, S = k.nc, k.S
    D = k.din
    for nm, shp in [("ident", [128, 128]), ("ones", [128, 128]), ("h1", [TOK, 1024]), ("affT_all", [128, 16, 64]), ("aff_own", [128, 16, 16]),
                    ("ln2_g", [1024]), ("ln2_b", [1024]), ("w_gate_e", [16, 1024, 1024]), ("w_up_e", [16, 1024, 1024]),
                    ("w_down_e", [16, 1024, 1024])]:
        k.inp(nm, shp)
    k.out("y", [TOK, 1024])
    if debug:
        k.out("dbg_gm", [128, 16, 16])
        k.out("dbg_acc", [128, 16, 1024])
        k.out("dbg_lo", [128, 16])
        for nm_ in ("dbg_wg", "dbg_wu", "dbg_wd"):
            k.out(nm_, [128, 8, 1024], BF16)
        k.out("dbg_hT0", [128, 8, 512], BF16)
        k.out("dbg_hT1", [128, 8, 512], BF16)
        k.out("dbg_h1T", [128, 8, TOK], BF16)
    V = lambda fn, r, w: S.op("dve", fn, reads=r, writes=w)
    A = lambda fn, r, w: S.op("act", fn, reads=r, writes=w)
    G = lambda fn, r, w: S.op("pool", fn, reads=r, writes=w)
    T = lambda fn, r, w: S.op("pe", fn, reads=r, writes=w)
    with k.top:
        st = k.top
        ident_f = k.sb(st, [128, 128]); ident_b = k.sb(st, [128, 128], BF16); ones = k.sb(st, [128, 128])
        gm = k.sb(st, [128, 16, 16])
        h1T = k.sb(st, [128, 8, TOK], BF16)
        acc = k.sb(st, [128, 16, 1024])
        bc_, bgm, bh1T, bacc = Buf(), Buf(), Buf(), Buf()
        S.dma("sp", "c", lambda e: e.dma_start(out=ident_f[:], in_=D["ident"]), writes=[bc_])
        S.dma("sp", "c", lambda e: e.dma_start(out=ones[:], in_=D["ones"]), writes=[bc_])
        A(lambda e: e.copy(out=ident_b[:], in_=ident_f[:]), [bc_], [bc_])
        for t_ in range(16):
            V(lambda e, t_=t_: e.memset(acc[:, t_, :], 0.0), [], [bacc])
        with contextlib.ExitStack() as st2:
            affT = k.sb(st2, [128, 16, 64]); cmp = k.sb(st2, [128, 16, 64]); affo = k.sb(st2, [128, 16, 16])
            lo = k.sb(st2, [128, 16]); hi = k.sb(st2, [128, 16]); mid = k.sb(st2, [128, 16]); cnt = k.sb(st2, [128, 16])
            ge = k.sb(st2, [128, 16]); d1 = k.sb(st2, [128, 16]); d2 = k.sb(st2, [128, 16])
            pC = k.ps(st2)
            bb, bpC = Buf(), Buf()
            S.dma("sp", "c", lambda e: e.dma_start(out=affT[:], in_=D["affT_all"]), writes=[bb])
            S.dma("sp", "c", lambda e: e.dma_start(out=affo[:], in_=D["aff_own"]), writes=[bb])
            V(lambda e: e.memset(lo[:], 0.0), [], [bb])
            V(lambda e: e.memset(hi[:], 1.0), [], [bb])
            for it in range(NBIS):
                V(lambda e: e.tensor_tensor(out=mid[:], in0=lo[:], in1=hi[:], op=ALU.add), [bb], [bb])
                V(lambda e: e.tensor_scalar(out=mid[:], in0=mid[:], scalar1=0.5, scalar2=None, op0=ALU.mult), [bb], [bb])
                V(lambda e: e.tensor_tensor(out=cmp[:], in0=affT[:], in1=bc(mid[:].unsqueeze(2), [128, 16, 64]), op=ALU.is_ge), [bb], [bb])
                V(lambda e: e.tensor_reduce(out=cnt[:], in_=cmp[:], axis=AX.X, op=ALU.add), [bb], [bb])
                T(lambda e: e.matmul(pC[:, 0:16], lhsT=ones[:], rhs=cnt[:], start=True, stop=True), [bb, bc_], [bpC])
                V(lambda e: e.tensor_scalar(out=ge[:], in0=pC[:, 0:16], scalar1=1023.5, scalar2=None, op0=ALU.is_ge), [bpC], [bb])
                V(lambda e: e.tensor_tensor(out=d1[:], in0=mid[:], in1=lo[:], op=ALU.subtract), [bb], [bb])
                V(lambda e: e.tensor_tensor(out=d1[:], in0=d1[:], in1=ge[:], op=ALU.mult), [bb], [bb])
                V(lambda e: e.tensor_tensor(out=lo[:], in0=lo[:], in1=d1[:], op=ALU.add), [bb], [bb])
                V(lambda e: e.tensor_tensor(out=d2[:], in0=hi[:], in1=mid[:], op=ALU.subtract), [bb], [bb])
                V(lambda e: e.tensor_tensor(out=d2[:], in0=d2[:], in1=ge[:], op=ALU.mult), [bb], [bb])
                V(lambda e: e.tensor_tensor(out=hi[:], in0=mid[:], in1=d2[:], op=ALU.add), [bb], [bb])
            V(lambda e: e.tensor_tensor(out=gm[:], in0=affo[:], in1=bc(lo[:].unsqueeze(1), [128, 16, 16]), op=ALU.is_ge), [bb], [bgm])
            V(lambda e: e.tensor_tensor(out=gm[:], in0=gm[:], in1=affo[:], op=ALU.mult), [bb, bgm], [bgm])
            if debug:
                S.dma("sp", "c", lambda e: e.dma_start(out=k.dout["dbg_gm"], in_=gm[:]), reads=[bgm], writes=[Buf()])
                S.dma("sp", "c", lambda e: e.dma_start(out=k.dout["dbg_lo"], in_=lo[:]), reads=[bb], writes=[Buf()])
            S.barrier()
        with contextlib.ExitStack() as st2:
            xt = [k.sb(st2, [128, 1024]) for _ in range(2)]
            xb = [k.sb(st2, [128, 1024], BF16) for _ in range(2)]
            pT = k.ps(st2, BF16)
            bx, bxb, bpT = [Buf(), Buf()], [Buf(), Buf()], Buf()
            for t in range(16):
                s_ = t % 2
                S.dma("sp", f"x{s_}", lambda e, t=t, s_=s_: e.dma_start(out=xt[s_][:], in_=D["h1"][t * 128:(t + 1) * 128, :]), writes=[bx[s_]])
                A(lambda e, s_=s_: e.copy(out=xb[s_][:], in_=xt[s_][:]), [bx[s_]], [bxb[s_]])
                for kb in range(8):
                    T(lambda e, kb=kb, s_=s_: e.transpose(out=pT[:, kb * 128:(kb + 1) * 128], in_=xb[s_][:, kb * 128:(kb + 1) * 128], identity=ident_b[:]),
                      [bxb[s_], bc_], [bpT])
                V(lambda e, t=t: e.tensor_copy(out=h1T[:, :, t * 128:(t + 1) * 128], in_=pT[:].rearrange("p (k n) -> p k n", k=8)), [bpT], [bh1T])
            S.barrier()
        with contextlib.ExitStack() as st2:
            wg = [k.sb(st2, [128, 8, 1024], BF16) for _ in range(2)]
            wu = [k.sb(st2, [128, 8, 1024], BF16) for _ in range(2)]
            wd = [k.sb(st2, [128, 8, 1024], BF16)]
            stg = [k.sb(st2, [128, 1024]) for _ in range(2)]
            hT = [k.sb(st2, [128, 8, 512], BF16) for _ in range(2)]
            slu = [k.sb(st2, [128, 512]) for _ in range(2)]
            pGt = [k.ps(st2) for _ in range(2)]
            pUp = [k.ps(st2) for _ in range(2)]
            pD = [k.ps(st2) for _ in range(2)]
            bwg, bwu, bwd = [Buf(), Buf()], [Buf(), Buf()], [Buf()]
            bstg = [Buf() for _ in range(2)]
            bhT, bslu = [Buf(), Buf()], [Buf(), Buf()]
            bpGt, bpUp, bpD = [Buf(), Buf()], [Buf(), Buf()], [Buf(), Buf()]
            ns = 0

            def load_w(dst, bdst, nm, e_):
                nonlocal ns
                for kb in range(8):
                    sl = ns % 2
                    ns += 1
                    S.dma("sp", f"ws{sl}", lambda e, sl=sl, kb=kb, nm=nm, e_=e_: e.dma_start(out=stg[sl][:], in_=D[nm][e_, kb * 128:(kb + 1) * 128, :]),
                          writes=[bstg[sl]])
                    G(lambda e, sl=sl, kb=kb, dst=dst: e.tensor_copy(out=dst[:, kb, :], in_=stg[sl][:]), [bstg[sl]], [bdst])
            nf = 0
            nd = 0
            nh = 0
            for ex in range(nexp):
                sl = ex % 2
                load_w(wg[sl], bwg[sl], "w_gate_e", ex)
                load_w(wu[sl], bwu[sl], "w_up_e", ex)
                load_w(wd[0], bwd[0], "w_down_e", ex)
                for tb in range(4):
                    hs = nh % 2
                    nh += 1
                    t0 = tb * 512
                    for ft in range(8):
                        ps_ = nf % 2
                        nf += 1
                        fs = slice(ft * 128, (ft + 1) * 128)
                        for kb in range(8):
                            T(lambda e, kb=kb, fs=fs, sl=sl, t0=t0, ps_=ps_: e.matmul(pGt[ps_][:], lhsT=wg[sl][:, kb, fs], rhs=h1T[:, kb, t0:t0 + 512],
                                                                                      start=(kb == 0), stop=(kb == 7)), [bwg[sl], bh1T], [bpGt[ps_]])
                        for kb in range(8):
                            T(lambda e, kb=kb, fs=fs, sl=sl, t0=t0, ps_=ps_: e.matmul(pUp[ps_][:], lhsT=wu[sl][:, kb, fs], rhs=h1T[:, kb, t0:t0 + 512],
                                                                                      start=(kb == 0), stop=(kb == 7)), [bwu[sl], bh1T], [bpUp[ps_]])
                        A(lambda e, ps_=ps_: e.activation(out=slu[ps_][:], in_=pGt[ps_][:], func=AF.Sigmoid), [bpGt[ps_]], [bslu[ps_]])
                        V(lambda e, ps_=ps_: e.tensor_tensor(out=slu[ps_][:], in0=slu[ps_][:], in1=pGt[ps_][:], op=ALU.mult),
                          [bslu[ps_], bpGt[ps_]], [bslu[ps_]])
                        V(lambda e, ps_=ps_, hs=hs, ft=ft: e.tensor_tensor(out=hT[hs][:, ft, :], in0=slu[ps_][:], in1=pUp[ps_][:], op=ALU.mult),
                          [bslu[ps_], bpUp[ps_]], [bhT[hs]])
                    for tt in range(4):
                        tile_ = tb * 4 + tt
                        for dh in range(2):
                            pd_ = nd % 2
                            nd += 1
                            for kb in range(8):
                                T(lambda e, kb=kb, hs=hs, tt=tt, dh=dh, pd_=pd_: e.matmul(
                                    pD[pd_][:], lhsT=hT[hs][:, kb, tt * 128:(tt + 1) * 128], rhs=wd[0][:, kb, dh * 512:(dh + 1) * 512],
                                    start=(kb == 0), stop=(kb == 7)), [bhT[hs], bwd[0]], [bpD[pd_]])
                            V(lambda e, tile_=tile_, dh=dh, pd_=pd_, ex=ex: e.scalar_tensor_tensor(
                                out=acc[:, tile_, dh * 512:(dh + 1) * 512], in0=pD[pd_][:], scalar=(1.0 if unit_gate else gm[:, tile_, ex:ex + 1]),
                                in1=acc[:, tile_, dh * 512:(dh + 1) * 512], op0=ALU.mult, op1=ALU.add), [bpD[pd_], bgm, bacc], [bacc])
            if debug:
                S.barrier()
                for nm_, t_ in (("dbg_wg", wg[0]), ("dbg_wu", wu[0]), ("dbg_wd", wd[0]), ("dbg_hT0", hT[0]), ("dbg_hT1", hT[1]), ("dbg_h1T", h1T)):
                    S.dma("sp", "c", lambda e, nm_=nm_, t_=t_: e.dma_start(out=k.dout[nm_], in_=t_[:]), writes=[Buf()])
            S.barrier()
        if debug:
            S.dma("sp", "c", lambda e: e.dma_start(out=k.dout["dbg_acc"], in_=acc[:]), reads=[bacc], writes=[Buf()])
            S.barrier()
        with contextlib.ExitStack() as st2:
            g_t = k.sb(st2, [128, 1024]); b_t = k.sb(st2, [128, 1024])
            xl = [k.sb(st2, [128, 1024]) for _ in range(2)]
            stt = k.sb(st2, [128, 2, 6]); mv = k.sb(st2, [128, 2]); rstd = k.sb(st2, [128, 1])
            bgb, bxl, bstat, bo = Buf(), [Buf(), Buf()], Buf(), Buf()
            S.dma("sp", "c", lambda e: e.dma_start(out=g_t[:], in_=D["ln2_g"].partition_broadcast(128)), writes=[bgb])
            S.dma("sp", "c", lambda e: e.dma_start(out=b_t[:], in_=D["ln2_b"].partition_broadcast(128)), writes=[bgb])
            for t in range(16):
                s_ = t % 2
                S.dma("sp", f"x{s_}", lambda e, t=t, s_=s_: e.dma_start(out=xl[s_][:], in_=D["h1"][t * 128:(t + 1) * 128, :]), writes=[bxl[s_]])
                V(lambda e, t=t, s_=s_: e.scalar_tensor_tensor(out=xl[s_][:], in0=xl[s_][:], scalar=ALPHA, in1=acc[:, t, :], op0=ALU.mult, op1=ALU.add),
                  [bxl[s_], bacc], [bxl[s_]])
                ln_tile(S, k, xl[s_], stt, mv, rstd, g_t, b_t, bxl[s_], bstat, bgb)
                S.dma("sp", f"o{s_}", lambda e, t=t, s_=s_: e.dma_start(out=k.dout["y"][t * 128:(t + 1) * 128, :], in_=xl[s_][:]),
                      reads=[bxl[s_]], writes=[bo])
            S.barrier()
        with nc.Block() as block:
            S.emit(block)
    return k


def l3_inputs(inputs, res2):
    out = []
    for c in range(8):
        b = c // 4
        aff_all = np.concatenate([res2[b * 4 + r]["aff"] for r in range(4)], axis=0)
        m = {
            "ident": np.eye(128, dtype=np.float32), "ones": np.ones((128, 128), np.float32),
            "h1": res2[c]["h1"],
            "affT_all": np.ascontiguousarray(aff_all.reshape(128, 64, 16).transpose(0, 2, 1)),
            "aff_own": np.ascontiguousarray(res2[c]["aff"].reshape(16, 128, 16).transpose(1, 0, 2)),
            "ln2_g": inputs["ln2_g"][0], "ln2_b": inputs["ln2_b"][0],
            "w_gate_e": inputs["w_gate_e"][0], "w_up_e": inputs["w_up_e"][0], "w_down_e": inputs["w_down_e"][0],
        }
        out.append(m)
    return out


_CACHE = {}


def _get(stage):
    if stage not in _CACHE:
        _CACHE[stage] = build_l3() if stage == "L3" else build(stage)
    return _CACHE[stage]


def kernel(**inputs):
    inputs = {k_: np.asarray(v) for k_, v in inputs.items()}
    cores = list(range(8))
    maps1 = [{**common_inputs(inputs, c), **ssm_inputs(inputs, c)} for c in cores]
    r1 = run_bass_kernel_spmd(_get("L1").nc, maps1, core_ids=cores).results
    maps2 = l2_inputs(inputs, maps1, r1)
    r2 = run_bass_kernel_spmd(_get("L2").nc, maps2, core_ids=cores).results
    maps3 = l3_inputs(inputs, r2)
    r3 = run_bass_kernel_spmd(_get("L3").nc, maps3, core_ids=cores).results
    out = np.zeros((2, 8192, 1024), np.float32)
    for c in cores:
        b, r = c // 4, c % 4
        out[b, r * TOK:(r + 1) * TOK] = r3[c]["y"]
    return out
```
